# Optimizing a Trainium2 kernel written in Bass

```python
import jax, jax.numpy as jnp
from jax import lax
import numpy as np

D_MODEL = 1024
BATCH = 8
SEQ = 2048
DEPTH = 2

GRID_W = 64
CTX_LEN = 256
HEAD_DIM = 64
EPS = 1e-6
A_WIDTH = D_MODEL // 4
A_HEADS = A_WIDTH // HEAD_DIM
A_CHUNK = 128
B_WIDTH = D_MODEL // 2
B_HEADS = B_WIDTH // HEAD_DIM
B_KV_HEADS = 2
B_GROUP = B_HEADS // B_KV_HEADS
B_KV_WIDTH = B_KV_HEADS * HEAD_DIM
Q_BLOCK = 128
ROPE_AXIS_DIM = HEAD_DIM // 2
ROPE_BASE = 10000.0
C_WIDTH = D_MODEL // 4
C_HEADS = C_WIDTH // HEAD_DIM
C_CHUNK = 64
MIX_WIDTH = A_WIDTH + B_WIDTH + C_WIDTH
OFF_B = 2 * A_WIDTH
OFF_KV = OFF_B + B_WIDTH
OFF_C = OFF_KV + 2 * B_KV_WIDTH
OFF_G = OFF_C + 4 * C_WIDTH
IN_WIDTH = OFF_G + C_WIDTH
N_GROUPS = 4
EXPERTS_PER_GROUP = 8
N_EXPERTS = N_GROUPS * EXPERTS_PER_GROUP
TOP_K = 2
D_FF_EXPERT = D_MODEL // 2
MOE_BLOCK = 128

kernel_name = 'hybrid_dit_gmlp_gqa_hgrn2_hmoe'


def _rms(x):
    xf = x.astype(jnp.float32)
    return (xf * lax.rsqrt(jnp.mean(xf * xf, axis=-1, keepdims=True) + EPS)).astype(x.dtype)


def axial_rope_tables(rows):
    row = jnp.repeat(jnp.arange(rows), GRID_W).astype(jnp.float32)
    col = jnp.tile(jnp.arange(GRID_W), rows).astype(jnp.float32)
    inv_freq = 1.0 / (ROPE_BASE ** (jnp.arange(0, ROPE_AXIS_DIM, 2, dtype=jnp.float32) / ROPE_AXIS_DIM))
    ang = jnp.stack([row[:, None] * inv_freq, col[:, None] * inv_freq], axis=1)
    return jnp.cos(ang)[:, None, :, None, :], jnp.sin(ang)[:, None, :, None, :]


def apply_rope(x, cos, sin):
    b, t, h, _ = x.shape
    xr = x.astype(jnp.float32).reshape(b, t, h, 2, 2, ROPE_AXIS_DIM // 2)
    rot = jnp.stack([-xr[..., 1, :], xr[..., 0, :]], axis=-2)
    return (xr * cos + rot * sin).reshape(b, t, h, HEAD_DIM).astype(x.dtype)


def chunk_mlp(z, w_s, b_s):
    b, t, _ = z.shape
    u, v = jnp.split(jax.nn.gelu(z), 2, axis=-1)
    v = _rms(v.reshape(b, t // A_CHUNK, A_CHUNK, A_HEADS, HEAD_DIM))
    mixed = jnp.einsum('hts,bnshd->bnthd', w_s, v) + b_s.T[:, :, None]
    return u * mixed.reshape(b, t, A_WIDTH)


def _gqa(q, k, v):
    s = jnp.einsum('bqkgd,bskd->bkgqs', q, k).astype(jnp.float32) * (HEAD_DIM ** -0.5)
    p = jax.nn.softmax(s, axis=-1).astype(v.dtype)
    return jnp.einsum('bkgqs,bskd->bqkgd', p, v)


def latent_attention(q, k, v, kc, vc):
    b, t, _, _ = q.shape
    keys = jnp.concatenate([k, kc], axis=1)
    vals = jnp.concatenate([v, vc], axis=1)
    qb = q.reshape(b, t // Q_BLOCK, Q_BLOCK, B_KV_HEADS, B_GROUP, HEAD_DIM).transpose(1, 0, 2, 3, 4, 5)
    out = lax.map(lambda qblk: _gqa(qblk, keys, vals), qb)
    return out.transpose(1, 0, 2, 3, 4, 5).reshape(b, t, B_WIDTH)


def forget_gate(zf, lb):
    zf32 = zf.astype(jnp.float32)
    lbh = lb.reshape(C_HEADS, HEAD_DIM)
    pos = lbh > 0.0
    log_lb = jnp.log(jnp.where(pos, lbh, 1.0))
    log_rest = jnp.log1p(-lbh) + jax.nn.log_sigmoid(zf32)
    log_f = jnp.where(pos, jnp.logaddexp(log_lb, log_rest), log_rest)
    k = (1.0 - lbh) * jax.nn.sigmoid(-zf32)
    return k, log_f


def hgrn_inputs(z, lb):
    b, t, _ = z.shape
    q, zf, zb, v = [a.reshape(b, t, C_HEADS, HEAD_DIM) for a in jnp.split(z, 4, axis=-1)]
    kf, logf_f = forget_gate(zf, lb[0])
    kb, logf_b = forget_gate(zb, lb[1])
    return jax.nn.silu(q), v, kf, logf_f, kb, logf_b


def hgrn_scan(q, k, v, log_f, s0, with_output):
    b, t, h, _ = q.shape
    nc = t // C_CHUNK

    def chunks(a):
        return a.astype(jnp.float32).reshape(b, nc, C_CHUNK, h, a.shape[-1]).transpose(1, 0, 3, 2, 4)

    qc, kc, vc = chunks(q), chunks(k), chunks(v)
    bc = jnp.cumsum(chunks(log_f), axis=3)
    tri = jnp.tril(jnp.ones((C_CHUNK, C_CHUNK), bool))[:, :, None]

    def step(s, inp):
        qi, ki, vi, bi = inp
        b_last = bi[:, :, -1:, :]
        s_new = jnp.exp(b_last[:, :, 0, :, None]) * s + jnp.einsum('bhsk,bhsv->bhkv', ki * jnp.exp(b_last - bi), vi)
        if not with_output:
            return s_new, None
        diff = bi[:, :, :, None, :] - bi[:, :, None, :, :]
        decay = jnp.where(tri, jnp.exp(jnp.where(tri, diff, 0.0)), 0.0)
        scores = jnp.einsum('bhtk,bhsk,bhtsk->bhts', qi, ki, decay)
        o = jnp.einsum('bhts,bhsv->bhtv', scores, vi) + jnp.einsum('bhtk,bhkv->bhtv', qi * jnp.exp(bi), s)
        return s_new, o

    s_fin, o = lax.scan(step, s0, (qc, kc, vc, bc))
    if with_output:
        o = o.transpose(1, 0, 3, 2, 4).reshape(b, t, h, v.shape[-1])
    return s_fin, o


def hgrn_bidir(z, zc, lb, ctx_out):
    q, v, kf, lf, kb, lbw = hgrn_inputs(z, lb)
    qc, vc, kfc, lfc, kbc, lbc = hgrn_inputs(zc, lb)
    s0 = jnp.zeros((z.shape[0], C_HEADS, HEAD_DIM, HEAD_DIM), jnp.float32)
    rev = lambda a: a[:, ::-1]
    sf, of_c = hgrn_scan(qc, kfc, vc, lfc, s0, ctx_out)
    _, of = hgrn_scan(q, kf, v, lf, sf, True)
    sb, ob_c = hgrn_scan(rev(qc), rev(kbc), rev(vc), rev(lbc), s0, ctx_out)
    _, ob = hgrn_scan(rev(q), rev(kb), rev(v), rev(lbw), sb, True)
    o = of + rev(ob)
    o_c = of_c + rev(ob_c) if ctx_out else None
    return o, o_c


def hgrn_out(o, g, w):
    b, t = g.shape[:2]
    return (_rms(o) * w).reshape(b, t, C_WIDTH).astype(g.dtype) * jax.nn.silu(g)


def hier_moe(h, w_grp, b_grp, w_exp, b_exp, w1, w3, w2):
    n_tok, d = h.shape
    g_logits = (h @ w_grp + b_grp).astype(jnp.float32)
    grp = jnp.argmax(g_logits, axis=-1)
    p_grp = jnp.take_along_axis(jax.nn.softmax(g_logits, axis=-1), grp[:, None], axis=-1)
    e_logits = (h @ w_exp + b_exp).astype(jnp.float32).reshape(n_tok, N_GROUPS, EXPERTS_PER_GROUP)
    e_in = jnp.take_along_axis(e_logits, grp[:, None, None], axis=1)[:, 0]
    top_val, top_idx = lax.top_k(e_in, TOP_K)
    gate = (jax.nn.softmax(top_val, axis=-1) * p_grp).reshape(-1)
    expert = (grp[:, None] * EXPERTS_PER_GROUP + top_idx).reshape(-1)
    tok = jnp.repeat(jnp.arange(n_tok), TOP_K)
    n_asg = n_tok * TOP_K
    order = jnp.argsort(expert)
    e_sorted, tok_sorted, gate_sorted = expert[order], tok[order], gate[order]
    counts = jax.ops.segment_sum(jnp.ones_like(expert), expert, num_segments=N_EXPERTS)
    padded = (counts + MOE_BLOCK - 1) // MOE_BLOCK * MOE_BLOCK
    starts = jnp.cumsum(counts) - counts
    pad_ends = jnp.cumsum(padded)
    pad_starts = pad_ends - padded
    dest = pad_starts[e_sorted] + jnp.arange(n_asg) - starts[e_sorted]
    n_blocks = -(-n_asg // MOE_BLOCK) + N_EXPERTS
    buf = jnp.zeros((n_blocks * MOE_BLOCK, d), h.dtype).at[dest].set(h[tok_sorted])
    blk_expert = jnp.minimum(jnp.searchsorted(pad_ends, jnp.arange(n_blocks) * MOE_BLOCK, side='right'), N_EXPERTS - 1)

    def expert_block(args):
        xb, e = args
        return (jax.nn.silu(xb @ w1[e]) * (xb @ w3[e])) @ w2[e]

    out = lax.map(expert_block, (buf.reshape(n_blocks, MOE_BLOCK, d), blk_expert)).reshape(-1, d)
    y = jax.ops.segment_sum(out[dest] * gate_sorted[:, None], tok_sorted, num_segments=n_tok)
    return y.astype(h.dtype)


def hybrid_layer(x, xc, c, c_ctx, cos, sin, lb, w_mod, b_mod, norm1_w, w_in, w_s, b_s, q_norm_w, k_norm_w,
                 hgrn_norm_w, w_out, norm2_w, w_grp, b_grp, w_exp, b_exp, w1, w3, w2, ctx_out):
    b, t, d = x.shape
    lc = xc.shape[1]
    sh1, sc1, g1, sh2, sc2, g2 = jnp.split((jax.nn.silu(c) @ w_mod + b_mod)[:, None, :], 6, axis=-1)
    n_mod = 6 if ctx_out else 2
    mod_c = jnp.split(jax.nn.silu(c_ctx) @ w_mod[:, :n_mod * d] + b_mod[:n_mod * d], n_mod)
    h = _rms(x) * norm1_w * (1.0 + sc1) + sh1
    hc = _rms(xc) * norm1_w * (1.0 + mod_c[1]) + mod_c[0]
    p = h @ w_in
    pc = hc @ w_in[:, OFF_KV:OFF_G]
    ya = chunk_mlp(p[..., :OFF_B], w_s, b_s)
    q = apply_rope(_rms(p[..., OFF_B:OFF_KV].reshape(b, t, B_HEADS, HEAD_DIM)) * q_norm_w, cos, sin)
    k, v = jnp.split(p[..., OFF_KV:OFF_C].reshape(b, t, 2 * B_KV_HEADS, HEAD_DIM), 2, axis=2)
    k = apply_rope(_rms(k) * k_norm_w, cos, sin)
    kc, vc = jnp.split(pc[..., :2 * B_KV_WIDTH].reshape(b, lc, 2 * B_KV_HEADS, HEAD_DIM), 2, axis=2)
    kc = _rms(kc) * k_norm_w
    yb = latent_attention(q, k, v, kc, vc)
    o, o_c = hgrn_bidir(p[..., OFF_C:OFF_G], pc[..., 2 * B_KV_WIDTH:], lb, ctx_out)
    yc = hgrn_out(o, p[..., OFF_G:], hgrn_norm_w)
    x = x + g1 * (jnp.concatenate([ya, yb, yc], axis=-1) @ w_out)
    h2 = _rms(x) * norm2_w * (1.0 + sc2) + sh2
    if ctx_out:
        pc_rest = hc @ w_in[:, :OFF_KV]
        pc_g = hc @ w_in[:, OFF_G:]
        ya_c = chunk_mlp(pc_rest[..., :OFF_B], w_s, b_s)
        qc = _rms(pc_rest[..., OFF_B:].reshape(b, lc, B_HEADS, HEAD_DIM)) * q_norm_w
        yb_c = _gqa(qc.reshape(b, lc, B_KV_HEADS, B_GROUP, HEAD_DIM), kc, vc).reshape(b, lc, B_WIDTH)
        yc_c = hgrn_out(o_c, pc_g, hgrn_norm_w)
        xc = xc + mod_c[2] * (jnp.concatenate([ya_c, yb_c, yc_c], axis=-1) @ w_out)
        h2c = _rms(xc) * norm2_w * (1.0 + mod_c[4]) + mod_c[3]
        tokens = jnp.concatenate([h2.reshape(-1, d), h2c.reshape(-1, d)], axis=0)
        m = hier_moe(tokens, w_grp, b_grp, w_exp, b_exp, w1, w3, w2)
        x = x + g2 * m[:b * t].reshape(b, t, d)
        xc = xc + mod_c[5] * m[b * t:].reshape(b, lc, d)
    else:
        x = x + g2 * hier_moe(h2.reshape(-1, d), w_grp, b_grp, w_exp, b_exp, w1, w3, w2).reshape(b, t, d)
    return x, xc


def setup_inputs(seed: int = 0) -> dict:
    key = jax.random.key(seed)
    ks = jax.random.split(key, 24)
    nrm = lambda k, shape, scale: jax.random.normal(k, shape, jnp.float32) * scale
    return {
        'x': nrm(ks[0], (BATCH, SEQ, D_MODEL), 1.0),
        'c': nrm(ks[1], (BATCH, D_MODEL), 1.0),
        'ctx': nrm(ks[2], (BATCH, CTX_LEN, D_MODEL), 1.0),
        'c_ctx': nrm(ks[3], (D_MODEL,), 1.0),
        'w_mod': nrm(ks[4], (DEPTH, D_MODEL, 6 * D_MODEL), 0.5 * D_MODEL ** -0.5),
        'b_mod': nrm(ks[5], (DEPTH, 6 * D_MODEL), 0.02),
        'norm1_w': 1.0 + nrm(ks[6], (DEPTH, D_MODEL), 0.02),
        'w_in': nrm(ks[7], (DEPTH, D_MODEL, IN_WIDTH), D_MODEL ** -0.5),
        'w_s': nrm(ks[8], (DEPTH, A_HEADS, A_CHUNK, A_CHUNK), A_CHUNK ** -0.5),
        'b_s': 1.0 + nrm(ks[9], (DEPTH, A_HEADS, A_CHUNK), 0.02),
        'q_norm_w': 1.0 + nrm(ks[10], (DEPTH, HEAD_DIM), 0.02),
        'k_norm_w': 1.0 + nrm(ks[11], (DEPTH, HEAD_DIM), 0.02),
        'hgrn_lb_logits': nrm(ks[12], (DEPTH, 2, C_WIDTH), 1.0),
        'hgrn_norm_w': 1.0 + nrm(ks[13], (DEPTH, HEAD_DIM), 0.02),
        'w_out': nrm(ks[14], (DEPTH, MIX_WIDTH, D_MODEL), MIX_WIDTH ** -0.5),
        'norm2_w': 1.0 + nrm(ks[15], (DEPTH, D_MODEL), 0.02),
        'w_grp': nrm(ks[16], (DEPTH, D_MODEL, N_GROUPS), D_MODEL ** -0.5),
        'b_grp': nrm(ks[17], (DEPTH, N_GROUPS), 0.01),
        'w_exp': nrm(ks[18], (DEPTH, D_MODEL, N_EXPERTS), D_MODEL ** -0.5),
        'b_exp': nrm(ks[19], (DEPTH, N_EXPERTS), 0.01),
        'w1': nrm(ks[20], (DEPTH, N_EXPERTS, D_MODEL, D_FF_EXPERT), D_MODEL ** -0.5),
        'w3': nrm(ks[21], (DEPTH, N_EXPERTS, D_MODEL, D_FF_EXPERT), D_MODEL ** -0.5),
        'w2': nrm(ks[22], (DEPTH, N_EXPERTS, D_FF_EXPERT, D_MODEL), D_FF_EXPERT ** -0.5),
    }


def reference(x, c, ctx, c_ctx, w_mod, b_mod, norm1_w, w_in, w_s, b_s, q_norm_w, k_norm_w, hgrn_lb_logits,
              hgrn_norm_w, w_out, norm2_w, w_grp, b_grp, w_exp, b_exp, w1, w3, w2):
    ROWS = x.shape[1] // GRID_W
    cos, sin = axial_rope_tables(ROWS)
    lb_sm = jax.nn.softmax(hgrn_lb_logits.astype(jnp.float32), axis=0)
    lb = jnp.cumsum(lb_sm, axis=0) - lb_sm[0]
    xc = ctx
    for l in range(DEPTH):
        x, xc = hybrid_layer(x, xc, c, c_ctx, cos, sin, lb[l], w_mod[l], b_mod[l], norm1_w[l], w_in[l], w_s[l],
                             b_s[l], q_norm_w[l], k_norm_w[l], hgrn_norm_w[l], w_out[l], norm2_w[l], w_grp[l],
                             b_grp[l], w_exp[l], b_exp[l], w1[l], w3[l], w2[l], ctx_out=(l < DEPTH - 1))
    return x
```

```python
import os
import numpy as np
from contextlib import ExitStack
import ml_dtypes
import concourse.bass as bass
import concourse.mybir as mybir
from concourse.bass_utils import run_bass_kernel_spmd

F32 = mybir.dt.float32
F32R = mybir.dt.float32r
BF16 = mybir.dt.bfloat16
AF = mybir.ActivationFunctionType
ALU = mybir.AluOpType
AX = mybir.AxisListType

D = 1024
NT = 18
NTOK = NT * 128
DEPTH = 2
INW = 2560
EPS = 1e-6
NE = 32
DFF = 512
HGC = 3072
CAP = 512
U32 = mybir.dt.uint32


class Buf:
    __slots__ = ("name", "st")

    def __init__(self, name):
        self.name = name
        self.st = {}


class V:
    __slots__ = ("buf", "key", "ap")

    def __init__(self, buf, key, ap):
        self.buf = buf
        self.key = key
        self.ap = ap

    def __getitem__(self, i):
        return V(self.buf, self.key, self.ap[i])

    def k(self, key):
        return V(self.buf, key, self.ap)

    def bc(self, dt):
        return V(self.buf, self.key, self.ap.bitcast(dt))

    def re(self, pat, **kw):
        return V(self.buf, self.key, self.ap.rearrange(pat, **kw))

    def tb(self, shape):
        return V(self.buf, self.key, self.ap.to_broadcast(shape))


class Op:
    __slots__ = ("eng", "n", "sem", "val", "ep")


class Kern:
    ENG = ["pe", "act", "dve", "pool", "sp"]

    def __init__(self, nc):
        self.nc = nc
        self.e = {"pe": nc.tensor, "act": nc.scalar, "dve": nc.vector, "pool": nc.gpsimd, "sp": nc.sync}
        self.sem = {n: nc.alloc_semaphore("es_" + n) for n in self.ENG}
        self.cnt = {n: 0 for n in self.ENG}
        self.waited = {n: {} for n in self.ENG}
        self.dq = {"sp": [nc.alloc_semaphore("dsp%d" % i) for i in range(40)],
                   "pool": [nc.alloc_semaphore("dpl%d" % i) for i in range(12)],
                   "act": [nc.alloc_semaphore("dac%d" % i) for i in range(8)]}
        self.dqi = {"sp": 0, "pool": 0, "act": 0}
        self.dqv = {}
        self.nops = 0
        self.g1 = nc.alloc_semaphore("gate1")
        self.g2 = nc.alloc_semaphore("gate2")
        self.nreset = 0
        self.noinc_ok = True
        self.uid = 0
        self.pending = None
        self._rec = None

    def sb(self, es, name, shape, dt):
        self.uid += 1
        name = "%s_u%d" % (name, self.uid)
        h = es.enter_context(self.nc.sbuf_tensor(name, list(shape), dt))
        return V(Buf(name), None, h[:])

    def ps(self, es, name, shape, dt=F32):
        self.uid += 1
        name = "%s_u%d" % (name, self.uid)
        h = es.enter_context(self.nc.psum_tensor(name, list(shape), dt))
        return V(Buf(name), None, h[:])

    def dram(self, name, shape, dt, kind="Internal"):
        h = self.nc.dram_tensor(name, list(shape), dt, kind=kind)
        return V(Buf(name), None, h.ap())

    @staticmethod
    def _ents(v):
        st = v.buf.st
        if v.key is None:
            return list(st.values())
        r = []
        if v.key in st:
            r.append(st[v.key])
        if None in st:
            r.append(st[None])
        return r

    def _collect(self, reads, writes):
        deps = []
        for v in reads:
            for ent in self._ents(v):
                if ent[0] is not None:
                    deps.append(ent[0])
        for v in writes:
            for ent in self._ents(v):
                if ent[0] is not None:
                    deps.append(ent[0])
                deps.extend(ent[1].values())
        return deps

    def _register(self, op, reads, writes):
        for v in reads:
            st = v.buf.st
            if v.key not in st:
                st[v.key] = [None, {}]
            rk = op.eng if op.sem is None else ("d", id(op.sem))
            st[v.key][1][rk] = op
        for v in writes:
            st = v.buf.st
            if v.key is None:
                st.clear()
            st[v.key] = [op, {}]

    def _wait(self, eng, sem, val):
        w = self.waited[eng]
        k = id(sem)
        if w.get(k, 0) >= val:
            return
        w[k] = val
        if self.pending is not None:
            self.pending.append((sem, val))
        else:
            self.e[eng].wait_ge(sem, val)

    def rec_start(self):
        self._rec = []

    def rec_stop(self):
        r = self._rec
        self._rec = None
        return r

    def emit_interleaved(self, lists, lag=4):
        if len(lists) > 2:
            ls = [list(x) for x in lists if x]
            while ls:
                for x in list(ls):
                    o_ = x.pop(0)
                    self.op(*o_[0], **o_[1])
                    if not x:
                        ls.remove(x)
            return
        a = lists[0]
        b = lists[1] if len(lists) > 1 else []
        i = j = 0
        while i < len(a) or j < len(b):
            if i < len(a):
                self.op(*a[i][0], **a[i][1])
                i += 1
            if j < len(b) and (i - j > lag or i >= len(a)):
                self.op(*b[j][0], **b[j][1])
                j += 1

    def op(self, eng, fn, reads=(), writes=(), dma=False, noinc=False, multi=False):
        if self._rec is not None:
            self._rec.append(((eng, fn, reads, writes), dict(dma=dma, noinc=noinc, multi=multi)))
            return None
        reads = [v for v in reads if isinstance(v, V)]
        writes = [v for v in writes if isinstance(v, V)]
        deps = self._collect(reads, writes)
        attach = not (dma or multi or os.environ.get("KNOATTACH"))
        if attach:
            self.pending = []
        for d in deps:
            if d.ep != self.nreset:
                continue
            if d.sem is not None:
                self._wait(eng, d.sem, d.val)
            else:
                if d.eng == "pe" and eng == "pe" and not dma:
                    continue
                self._wait(eng, self.sem[d.eng], d.n)
        o = Op()
        o.ep = self.nreset
        o.eng = eng
        o.sem = None
        o.val = 0
        o.n = 0
        if dma:
            q = self.dq[eng]
            i = self.dqi[eng]
            self.dqi[eng] = (i + 1) % len(q)
            s = q[i]
            prev = self.dqv.get(id(s), 0)
            if prev:
                self._wait(eng, s, prev)
            ins = fn(self.e[eng])
            ins.then_inc(s, 16)
            self.dqv[id(s)] = prev + 16
            o.sem = s
            o.val = prev + 16
        else:
            last = None
            if attach:
                pend = self.pending
                self.pending = None
                for (sm_, vl_) in pend[:-1]:
                    self.e[eng].wait_ge(sm_, vl_)
                if pend:
                    last = pend[-1]
            ins = fn(self.e[eng])
            if last is not None:
                ins._wait_ge(last[0], last[1])
            if noinc:
                o.n = self.cnt[eng] + 1
            else:
                self.cnt[eng] += 1
                ins.then_inc(self.sem[eng], 1)
                o.n = self.cnt[eng]
        self.nops += 1
        self._register(o, reads, writes)
        return o

    def barrier(self, reset=False):
        for eng in self.ENG:
            for other in self.ENG:
                if other != eng and self.cnt[other]:
                    self._wait(eng, self.sem[other], self.cnt[other])
            for q in self.dq.values():
                for s in q:
                    v = self.dqv.get(id(s), 0)
                    if v:
                        self._wait(eng, s, v)
        if not reset:
            return
        self.nreset += 1
        for eng in self.ENG:
            self.e[eng].sem_inc(self.g1, 1)
        pool = self.e["pool"]
        pool.wait_ge(self.g1, 5 * self.nreset)
        for n in self.ENG:
            pool.sem_clear(self.sem[n])
        for q in self.dq.values():
            for s in q:
                if self.dqv.get(id(s), 0):
                    pool.sem_clear(s)
        pool.sem_inc(self.g2, 1)
        for eng in self.ENG:
            self.e[eng].wait_ge(self.g2, self.nreset)
        self.cnt = {n: 0 for n in self.ENG}
        self.waited = {n: {} for n in self.ENG}
        self.dqv = {}

    def mm(self, out, lhsT, rhs, start=True, stop=True):
        rd = [lhsT, rhs] + ([] if start else [out])
        return self.op("pe", lambda e: e.matmul(out.ap, lhsT.ap, rhs.ap, start=start, stop=stop), rd, [out],
                       noinc=(not stop) and self.noinc_ok and not os.environ.get('KNONOINC'))

    def tr(self, out, in_, ident):
        return self.op("pe", lambda e: e.transpose(out.ap, in_.ap, ident.ap), [in_, ident], [out])

    def act(self, out, in_, func, bias=0.0, scale=1.0, accum=None):
        rd = [in_, bias, scale]
        wr = [out] + ([accum] if accum is not None else [])
        b = bias.ap if isinstance(bias, V) else bias
        sc = scale.ap if isinstance(scale, V) else scale
        if accum is None:
            return self.op("act", lambda e: e.activation(out=out.ap, in_=in_.ap, func=func, bias=b, scale=sc), rd, wr)
        return self.op("act", lambda e: e.activation(out=out.ap, in_=in_.ap, func=func, bias=b, scale=sc,
                                                     accum_out=accum.ap), rd, wr, multi=True)

    def tt(self, eng, out, a, b, op):
        if eng == "pool" and not os.environ.get("KPOOLTT"):
            eng = "dve"
        return self.op(eng, lambda e: e.tensor_tensor(out=out.ap, in0=a.ap, in1=b.ap, op=op), [a, b], [out])

    def ts(self, eng, out, a, s1, s2, op0, op1=None):
        x1 = s1.ap if isinstance(s1, V) else s1
        x2 = s2.ap if isinstance(s2, V) else s2
        if op1 is None:
            return self.op(eng, lambda e: e.tensor_scalar(out=out.ap, in0=a.ap, scalar1=x1, scalar2=None, op0=op0),
                           [a, s1], [out])
        return self.op(eng, lambda e: e.tensor_scalar(out=out.ap, in0=a.ap, scalar1=x1, scalar2=x2, op0=op0, op1=op1),
                       [a, s1, s2], [out])

    def stt(self, eng, out, a, s, b, op0, op1):
        x = s.ap if isinstance(s, V) else s
        return self.op(eng, lambda e: e.scalar_tensor_tensor(out=out.ap, in0=a.ap, scalar=x, in1=b.ap, op0=op0, op1=op1),
                       [a, s, b], [out])

    def cp(self, eng, out, in_):
        if eng == "act":
            return self.op("act", lambda e: e.copy(out=out.ap, in_=in_.ap), [in_], [out])
        return self.op(eng, lambda e: e.tensor_copy(out=out.ap, in_=in_.ap), [in_], [out])

    def red(self, eng, out, in_, op=ALU.add):
        return self.op(eng, lambda e: e.tensor_reduce(out=out.ap, in_=in_.ap, axis=AX.X, op=op), [in_], [out])

    def recip(self, out, in_):
        return self.op("dve", lambda e: e.reciprocal(out=out.ap, in_=in_.ap), [in_], [out])

    def memset(self, eng, out, val):
        return self.op(eng, lambda e: e.memset(out.ap, val), [], [out])

    def dma(self, q, out, in_):
        return self.op(q, lambda e: e.dma_start(out=out.ap, in_=in_.ap), [in_], [out], dma=True)

    def asel(self, out, pattern, cm, cmp):
        return self.op("pool", lambda e: e.affine_select(out=out.ap, in_=out.ap, pattern=pattern, compare_op=cmp,
                                                         fill=0.0, base=0, channel_multiplier=cm), [out], [out], multi=True)

    def top8(self, out, in_):
        return self.op("dve", lambda e: e.max(out=out.ap, in_=in_.ap), [in_], [out])


def host_consts():
    c = {}
    c["ident"] = np.eye(128, dtype=np.float32)
    c["identb"] = np.eye(128, dtype=np.float32).astype(ml_dtypes.bfloat16)
    s = np.arange(128)[:, None]
    t = np.arange(128)[None, :]
    same = (s // 64) == (t // 64)
    TF = (same & (s <= t)).astype(np.float32)
    TB = (same & (s >= t)).astype(np.float32)
    BO = same.astype(np.float32)
    c["hmat"] = np.stack([TF - 0.5 * BO, BO - TF, TB - 0.5 * BO, BO - TB]).transpose(1, 0, 2).copy()
    ind = np.zeros((128, 2), np.float32)
    ind[:64, 0] = 1
    ind[64:, 1] = 1
    c["ind"] = ind
    ecr = np.zeros((128, 33), np.float32)
    ecr[:, :32] = (np.arange(32) * CAP)[None, :]
    ecr[:, 32] = np.arange(128)
    c["ecr"] = ecr
    c["ltm"] = (s < t).astype(np.float32)
    rows = 2048 // 64
    row = np.repeat(np.arange(rows), 64).astype(np.float32)
    col = np.tile(np.arange(64), rows).astype(np.float32)
    inv = (1.0 / (10000.0 ** (np.arange(0, 32, 2, dtype=np.float32) / 32.0))).astype(np.float32)
    ang = np.stack([row[:, None] * inv, col[:, None] * inv], axis=1).astype(np.float32)
    cos = np.concatenate([np.ones((256, 2, 16), np.float32), np.cos(ang)], 0).reshape(NT, 128, 32)
    sin = np.concatenate([np.zeros((256, 2, 16), np.float32), np.sin(ang)], 0).reshape(NT, 128, 32)
    c["rope"] = np.stack([cos, sin], 2).transpose(1, 0, 2, 3).reshape(128, NT * 64).astype(np.float32).copy()
    return c


def build(nc, n_layers=DEPTH, dbg=None):
    nc.dge_precook = False
    K = Kern(nc)
    xin = K.dram("xin", [NTOK, D], F32, "ExternalInput")
    ccT = K.dram("ccT", [128, 8, 2], F32, "ExternalInput")
    w_mod = K.dram("w_mod", [DEPTH, D, 6 * D], F32R, "ExternalInput")
    b_mod = K.dram("b_mod", [DEPTH, 6 * D], F32, "ExternalInput")
    norm1_w = K.dram("norm1_w", [DEPTH, D], F32, "ExternalInput")
    norm2_w = K.dram("norm2_w", [DEPTH, D], F32, "ExternalInput")
    w_in = K.dram("w_in", [DEPTH, D, INW], F32R, "ExternalInput")
    w_s = K.dram("w_s", [DEPTH, 4, 128, 128], F32, "ExternalInput")
    b_sT = K.dram("b_sT", [DEPTH, 128, 4], F32, "ExternalInput")
    qkh_w = K.dram("qkh_w", [DEPTH, 3, 64], F32, "ExternalInput")
    lbl = K.dram("lbl", [DEPTH, 512], F32, "ExternalInput")
    w_out = K.dram("w_out", [DEPTH, D, D], F32, "ExternalInput")
    w_r = K.dram("w_r", [DEPTH, D, 36], F32, "ExternalInput")
    b_r = K.dram("b_r", [DEPTH, 36], F32, "ExternalInput")
    w1 = K.dram("w1", [DEPTH, NE, D, DFF], F32R, "ExternalInput")
    w3 = K.dram("w3", [DEPTH, NE, D, DFF], F32R, "ExternalInput")
    w2 = K.dram("w2", [DEPTH, NE, DFF, D], F32R, "ExternalInput")
    c_ident = K.dram("ident", [128, 128], F32, "ExternalInput")
    c_identb = K.dram("identb", [128, 128], BF16, "ExternalInput")
    c_hmat = K.dram("hmat", [128, 4, 128], F32, "ExternalInput")
    c_ind = K.dram("ind", [128, 2], F32, "ExternalInput")
    c_rope = K.dram("rope", [128, NT * 64], F32, "ExternalInput")
    out = K.dram("out", [2048, D], F32, "ExternalOutput")
    modD = K.dram("modD", [2, 6 * D], F32)
    hgD = K.dram("hgD", [NT, 128, HGC], BF16)
    qTD = K.dram("qTD", [128, 4, NTOK], BF16)
    XS = K.dram("XS", [NE * CAP + 2 * NTOK, D], BF16)
    YS = K.dram("YS", [NE * CAP, D], BF16)
    c_ecr = K.dram("ecr", [128, 33], F32, "ExternalInput")
    c_ltm = K.dram("ltm", [128, 128], F32, "ExternalInput")
    dbgD = None
    if dbg is not None:
        dbgD = K.dram("dbg", list(dbg[1]), F32, "ExternalOutput")

    with ExitStack() as top:
        xs = K.sb(top, "xs", [128, NT, D], F32)
        ident = K.sb(top, "ident_s", [128, 128], F32)
        identb = K.sb(top, "identb_s", [128, 128], BF16)
        hmat = K.sb(top, "hmat_s", [128, 4, 128], F32)
        ind = K.sb(top, "ind_s", [128, 2], F32)
        onesf = K.sb(top, "onesf", [128, 128], F32)
        epsc = K.sb(top, "epsc", [128, 1], F32)
        K.dma("sp", ident, c_ident)
        K.dma("sp", identb, c_identb)
        K.dma("sp", hmat, c_hmat)
        K.dma("sp", ind, c_ind)
        K.memset("pool", onesf, 1.0)
        K.memset("pool", epsc, EPS)
        for t in range(NT):
            K.dma("sp", xs.k(t)[:, t, :], xin[t * 128:(t + 1) * 128, :])

        def dump(v2d, r0=0):
            K.dma("pool", dbgD[r0:r0 + v2d.ap.shape[0], 0:v2d.ap.shape[1]], v2d)

        for l in range(n_layers):
            tiles = list(range(NT)) if l < DEPTH - 1 else list(range(2, NT))
            K.noinc_ok = False
            with ExitStack() as ph:
                cs = K.sb(ph, "cs", [128, 8, 2], F32)
                csr = K.sb(ph, "csr", [128, 8, 2], F32R)
                wm = [K.sb(ph, "wm%d" % i, [128, 3072], F32R) for i in range(2)]
                mrow = K.sb(ph, "mrow", [2, 6 * D], F32)
                brow = K.sb(ph, "brow", [2, 6 * D], F32)
                nrow = K.sb(ph, "nrow", [2, 2, D], F32)
                mps = [K.ps(ph, "mps%d" % i, [2, 512]) for i in range(6)]
                K.dma("sp", cs, ccT)
                K.act(csr, cs, AF.Silu)
                K.dma("sp", brow, b_mod[l:l + 1, :].tb([2, 6 * D]))
                K.dma("sp", nrow[:, 0, :], norm1_w[l:l + 1, :].tb([2, D]))
                K.dma("sp", nrow[:, 1, :], norm2_w[l:l + 1, :].tb([2, D]))
                i = 0
                for half in range(2):
                    for kc in range(8):
                        w = wm[i % 2]
                        i += 1
                        K.dma("sp", w, w_mod[l, kc * 128:(kc + 1) * 128, half * 3072:(half + 1) * 3072])
                        for n in range(6):
                            K.mm(mps[n], csr[:, kc, :], w[:, n * 512:(n + 1) * 512], start=(kc == 0), stop=(kc == 7))
                    for n in range(6):
                        c0 = half * 3072 + n * 512
                        K.tt("dve", mrow[:, c0:c0 + 512], mps[n], brow[:, c0:c0 + 512], ALU.add)
                K.stt("dve", mrow[:, D:2 * D], mrow[:, D:2 * D], 1.0, nrow[:, 0, :], ALU.add, ALU.mult)
                K.stt("dve", mrow[:, 4 * D:5 * D], mrow[:, 4 * D:5 * D], 1.0, nrow[:, 1, :], ALU.add, ALU.mult)
                K.dma("sp", modD, mrow)
                K.barrier()

            K.noinc_ok = True

            def bload(dst, slot, ctx):
                K.dma("sp", dst, modD[ctx:ctx + 1, slot * D:(slot + 1) * D].tb([128, D]))

            with ExitStack() as mix:
                catA = K.sb(mix, "catA", [128, 2, NTOK], BF16)
                EF = K.sb(mix, "EF", [128, 2, 2, 2 * NT], F32)
                EH = K.sb(mix, "EH", [128, 2, 2, 2 * NT], F32)
                whb = K.sb(mix, "whb", [128, 3, 64], F32)
                K.dma("sp", whb.re("p a c -> p (a c)"), qkh_w[l:l + 1].re("o a c -> o (a c)").tb([128, 192]))
                with ExitStack() as ab:
                    kT = K.sb(mix, "kT", [128, NTOK], BF16)
                    Ve = K.sb(mix, "Ve", [128, NT, 2, 65], BF16)
                    with ExitStack() as ph:
                        A1 = K.sb(ph, "A1", [128, D], F32)
                        SH1 = K.sb(ph, "SH1", [128, D], F32)
                        rope = K.sb(ph, "rope_s", [128, NT, 2, 32], F32)
                        K.dma("sp", rope.re("p t a c -> p (t a c)"), c_rope)
                        wsn = K.sb(ph, "wsn", [128, 4, 128], F32)
                        wsT = K.sb(ph, "wsT", [128, 4, 128], F32R)
                        bsT = K.sb(ph, "bsT", [128, 4], F32)
                        lbB = K.sb(ph, "lbB", [128, 512], F32)
                        omlB = K.sb(ph, "omlB", [128, 512], F32)
                        hTb = K.sb(ph, "hTb", [128, 8, 256], F32R)
                        wc = [K.sb(ph, "wc%d" % i, [128, 8, 256], F32R) for i in range(2)]
                        hbuf = K.sb(ph, "hbuf", [128, D], F32)
                        t2 = K.sb(ph, "t2", [128, 512], F32)
                        SA = []
                        for q_ in range(2):
                            sc = {}
                            sc["zb"] = K.sb(ph, "zb%d" % q_, [128, 512], F32)
                            sc["t1"] = K.sb(ph, "t1_%d" % q_, [128, 512], F32)
                            sc["t3"] = K.sb(ph, "t3_%d" % q_, [128, 512], F32)
                            sc["vnr"] = K.sb(ph, "vnr%d" % q_, [128, 256], F32R)
                            sc["yab"] = K.sb(ph, "yab%d" % q_, [128, 256], BF16)
                            sc["qrb"] = K.sb(ph, "qrb%d" % q_, [128, 512], BF16)
                            sc["ss"] = K.sb(ph, "ss%d" % q_, [128, 8], F32)
                            sc["rs"] = K.sb(ph, "rs%d" % q_, [128, 8], F32)
                            sc["qst"] = K.sb(ph, "qst%d" % q_, [128, 4, 128], BF16)
                            sc["ptrb"] = K.ps(ph, "ptrb%d" % q_, [128, 4, 128], BF16)
                            sc["hg"] = K.sb(ph, "hgs%d" % q_, [128, HGC], BF16)
                            sc["kkb"] = K.sb(ph, "kkb%d" % q_, [128, 512], F32)
                            sc["t2"] = K.sb(ph, "t2_%d" % q_, [128, 512], F32)
                            sc["qeb"] = K.sb(ph, "qeb%d" % q_, [128, 256], BF16)
                            sc["keb"] = K.sb(ph, "keb%d" % q_, [128, 256], BF16)
                            sc["pd"] = K.ps(ph, "pd%d" % q_, [128, 512])
                            K.memset("pool", sc["hg"], 0.0)
                            SA.append(sc)
                        zb = SA[0]["zb"]; t1 = SA[0]["t1"]; t3 = SA[0]["t3"]; ss = SA[0]["ss"]; rs = SA[0]["rs"]
                        ptrb = SA[0]["ptrb"]
                        sqh = [K.sb(ph, "sqh%d" % i, [128, 256], F32) for i in range(2)]
                        logf = [K.sb(ph, "logf%d" % i, [128, 512], F32) for i in range(2)]
                        ppA = [K.ps(ph, "ppA%d" % i, [128, 512]) for i in range(2)]
                        ptr = K.ps(ph, "ptr", [128, 4, 128], F32)
                        pmx = K.ps(ph, "pmx", [128, 512])
                        if os.environ.get("KVERB"):
                            print("phase A sbuf remaining", nc.sbuf_bytes_remaining, flush=True)
                        K.memset("pool", Ve[:, :, :, 64:65], 1.0)
                        K.dma("sp", wsn, w_s[l].re("h t s -> t h s"))
                        K.dma("sp", bsT, b_sT[l])
                        for h in range(4):
                            K.tr(ptr[:, h, :], wsn[:, h, :], ident)
                        K.cp("act", wsT, ptr)
                        if l == 0:
                            K.memset("pool", lbB, 0.0)
                            K.memset("pool", omlB, 1.0)
                        else:
                            K.dma("sp", t1, lbl[1:2, :].tb([128, 512]))
                            K.dma("sp", t2, lbl[0:1, :].tb([128, 512]))
                            K.tt("dve", t1, t1, t2, ALU.subtract)
                            K.act(lbB, t1, AF.Sigmoid)
                            K.ts("dve", omlB, lbB, -1.0, 1.0, ALU.mult, ALU.add)

                        def headnorm(src, nh, w_idx, dst, S=None):
                            S = S or SA[0]
                            t3 = S["t3"]; ss = S["ss"]; rs = S["rs"]
                            K.tt("pool", t3[:, 0:nh * 64], src, src, ALU.mult)
                            K.red("dve", ss[:, 0:nh], t3[:, 0:nh * 64].re("p (h c) -> p h c", c=64))
                            K.act(rs[:, 0:nh], ss[:, 0:nh], AF.Sqrt, bias=epsc, scale=1.0 / 64)
                            K.recip(rs[:, 0:nh], rs[:, 0:nh])
                            K.tt("dve", dst.re("p (h c) -> p h c", c=64), src.re("p (h c) -> p h c", c=64),
                                 rs[:, 0:nh].re("p (h o) -> p h o", o=1).tb([128, nh, 64]), ALU.mult)
                            if w_idx is not None:
                                K.tt("pool", dst.re("p (h c) -> p h c", c=64), dst.re("p (h c) -> p h c", c=64),
                                     whb[:, w_idx:w_idx + 1, :].tb([128, nh, 64]), ALU.mult)

                        def rope_apply(src, nh, t, dst, S=None):
                            S = S or SA[0]
                            t3 = S["t3"]
                            sv = src.re("p (h a s j) -> p h a s j", a=2, s=2, j=16)
                            dv = dst.re("p (h a s j) -> p h a s j", a=2, s=2, j=16)
                            cB = rope[:, t, 0:1, :].re("p o (a j) -> p o a j", a=2).tb([128, nh, 2, 16])
                            sB = rope[:, t, 1:2, :].re("p o (a j) -> p o a j", a=2).tb([128, nh, 2, 16])
                            a1 = t3[:, 0:nh * 32].re("p (h a j) -> p h a j", a=2, j=16)
                            a2 = t3[:, 256:256 + nh * 32].re("p (h a j) -> p h a j", a=2, j=16)
                            K.tt("pool", a1, sv[:, :, :, 0, :], cB, ALU.mult)
                            K.tt("dve", a2, sv[:, :, :, 1, :], sB, ALU.mult)
                            K.tt("dve", dv[:, :, :, 0, :], a1, a2, ALU.subtract)
                            K.tt("pool", a1, sv[:, :, :, 1, :], cB, ALU.mult)
                            K.tt("dve", a2, sv[:, :, :, 0, :], sB, ALU.mult)
                            K.tt("dve", dv[:, :, :, 1, :], a1, a2, ALU.add)

                        blocks = [[2 * b, 2 * b + 1] for b in range(9)]
                        wci = 0
                        ppi = 0
                        def hchain(blk):
                            if blk[0] in (0, 2):
                                bload(A1, 1, 1 if blk[0] == 0 else 0)
                                bload(SH1, 0, 1 if blk[0] == 0 else 0)
                            for bi, t in enumerate(blk):
                                ctx = 1 if t < 2 else 0
                                xt = xs.k(t)[:, t, :]
                                K.act(hbuf, xt, AF.Square, accum=ss[:, 0:1])
                                K.act(rs[:, 0:1], ss[:, 0:1], AF.Sqrt, bias=epsc, scale=1.0 / D)
                                K.recip(rs[:, 0:1], rs[:, 0:1])
                                K.stt("dve", hbuf, xt, rs[:, 0:1], A1, ALU.mult, ALU.mult)
                                K.tt("pool", hbuf, hbuf, SH1, ALU.add)
                                for half in range(2):
                                    for j in range(4):
                                        kc = half * 4 + j
                                        K.tr(ptr[:, j, :], hbuf[:, kc * 128:(kc + 1) * 128], ident)
                                    K.cp("act" if half == 0 else "dve",
                                         hTb[:, half * 4:half * 4 + 4, bi * 128:(bi + 1) * 128], ptr)
                        hchain(blocks[0])
                        for bix, blk in enumerate(blocks):
                            for c in range(5):
                                wpc = []
                                for hf in range(2):
                                    w = wc[wci % 2]
                                    wci += 1
                                    wpc.append(w)
                                    K.dma("sp", w, w_in[l, :, c * 512 + hf * 256:c * 512 + (hf + 1) * 256].re("(k p) n -> p k n", p=128))
                                pps = []
                                for bi, t in enumerate(blk):
                                    pps.append(ppA[ppi % 2])
                                    ppi += 1
                                for hf in range(2):
                                    for bi, t in enumerate(blk):
                                        for kc in range(8):
                                            K.mm(pps[bi][:, hf * 256:(hf + 1) * 256], hTb[:, kc, bi * 128:(bi + 1) * 128], wpc[hf][:, kc, :],
                                                 start=(kc == 0), stop=(kc == 7))
                                def post03(c, t, bi, pp, S):
                                    tok = slice(t * 128, (t + 1) * 128)
                                    zb = S["zb"]; t1 = S["t1"]; vnr = S["vnr"]; yab = S["yab"]; qrb = S["qrb"]
                                    qst = S["qst"]; ptrb = S["ptrb"]
                                    pmh = pmx[:, bi * 256:(bi + 1) * 256]
                                    if c == 0:
                                        K.act(zb, pp, AF.Gelu)
                                        headnorm(zb[:, 256:512], 4, None, t1[:, 0:256], S)
                                        K.cp("dve", vnr, t1[:, 0:256])
                                        for h in range(4):
                                            K.mm(pmh[:, h * 64:(h + 1) * 64], wsT[:, h, :], vnr[:, h * 64:(h + 1) * 64])
                                        for h in range(4):
                                            K.stt("dve", yab[:, h * 64:(h + 1) * 64], pmh[:, h * 64:(h + 1) * 64],
                                                  bsT[:, h:h + 1], zb[:, h * 64:(h + 1) * 64], ALU.add, ALU.mult)
                                        for j in range(2):
                                            K.tr(ptrb[:, j, :], yab[:, j * 128:(j + 1) * 128], identb)
                                        K.cp("act", catA.k(("a", t))[:, 0:2, tok], ptrb[:, 0:2, :])
                                    elif c == 1:
                                        K.cp("act", zb.re("p (j hi c) -> p hi j c", hi=2, j=4),
                                             pp.re("p (hi j c) -> p hi j c", hi=2, j=4))
                                        headnorm(zb, 8, 0, t1, S)
                                        rope_apply(t1, 8, t, qrb, S)
                                        for j in range(4):
                                            K.tr(ptrb[:, j, :], qrb[:, j * 128:(j + 1) * 128], identb)
                                        K.cp("act", qst, ptrb)
                                        K.dma("pool", qTD.k(t)[:, :, tok], qst)
                                    elif c == 2:
                                        K.cp("act", zb[:, 0:128], pp[:, 0:128])
                                        K.cp("act", Ve.k(t)[:, t, :, 0:64], pp[:, 128:256].re("p (h c) -> p h c", c=64))
                                        K.act(sqh[t % 2], pp[:, 256:512], AF.Silu)
                                        headnorm(zb[:, 0:128], 2, 1, t1[:, 0:128], S)
                                        rope_apply(t1[:, 0:128], 2, t, qrb[:, 0:128], S)
                                        K.tr(ptrb[:, 0, :], qrb[:, 0:128], identb)
                                        K.cp("act", kT.k(t)[:, tok], ptrb[:, 0, :])
                                    else:
                                        K.act(t1, pp, AF.Sigmoid)
                                        K.tt("dve", t1, t1, omlB, ALU.mult)
                                        K.tt("pool", t1, t1, lbB, ALU.add)
                                        K.act(logf[t % 2], t1, AF.Ln)
                                if c < 4:
                                    lists = []
                                    for bi, t in enumerate(blk):
                                        K.rec_start()
                                        post03(c, t, bi, pps[bi], SA[bi])
                                        lists.append(K.rec_stop())
                                    K.emit_interleaved(lists, lag=1)
                                    continue
                                def post4(t, bi, pp, S):
                                    hgt = S["hg"]; kkb = S["kkb"]; t2 = S["t2"]; t3 = S["t3"]; qeb = S["qeb"]; keb = S["keb"]
                                    pd = S["pd"]; ptrb = S["ptrb"]
                                    pmr = pmx[:, bi * 256:bi * 256 + 4]
                                    K.cp("act", hgt.k("v")[:, 2560:2816], pp[:, 0:256])
                                    K.act(kkb, logf[t % 2], AF.Exp)
                                    K.ts("dve", kkb, kkb, -1.0, 1.0, ALU.mult, ALU.add)
                                    K.act(hgt.k("v")[:, 2816:3072], pp[:, 256:512], AF.Silu)
                                    for d in range(2):
                                        base = d * 1280
                                        lf = logf[t % 2][:, d * 256:(d + 1) * 256]
                                        kd = kkb[:, d * 256:(d + 1) * 256]
                                        K.mm(pd[:, 0:256], hmat[:, 2 * d, :], lf)
                                        K.mm(pd[:, 256:512], hmat[:, 2 * d + 1, :], lf)
                                        for g in range(2):
                                            K.mm(pmr[:, 2 * g:2 * g + 2], lf[:, g * 128:(g + 1) * 128], ind)
                                        K.act(t2[:, 0:256], pd[:, 0:256], AF.Exp)
                                        K.tt("dve", qeb, sqh[t % 2], t2[:, 0:256], ALU.mult)
                                        K.act(t2[:, 256:512], pd[:, 0:256], AF.Exp, scale=-1.0)
                                        K.tt("pool", keb, kd, t2[:, 256:512], ALU.mult)
                                        K.act(t3[:, 0:256], pd[:, 256:512], AF.Exp)
                                        K.tt("dve", hgt.k("k2%d" % d)[:, base + 1024:base + 1280], kd, t3[:, 0:256], ALU.mult)
                                        tv = pmr.re("p (g c) -> p g c", c=2)
                                        K.act(EF.k((d, t))[:, d, :, 2 * t:2 * t + 2], tv, AF.Exp)
                                        K.act(EH.k((d, t))[:, d, :, 2 * t:2 * t + 2], tv, AF.Exp, scale=0.5)
                                        for g in range(2):
                                            K.tr(ptrb[:, g, :], qeb[:, g * 128:(g + 1) * 128], identb)
                                            K.tr(ptrb[:, 2 + g, :], keb[:, g * 128:(g + 1) * 128], identb)
                                        hq = hgt.k("q%d" % d)
                                        K.cp("act", hq[:, base:base + 256].re("p (g c) -> p g c", c=128), ptrb[:, 0:2, :])
                                        K.cp("dve", hq[:, base + 256:base + 512].re("p (g c) -> p g c", c=128)[:, :, 0:64],
                                             ptrb[:, 0:2, 0:64])
                                        K.cp("dve", hq[:, base + 512:base + 768].re("p (g c) -> p g c", c=128)[:, :, 64:128],
                                             ptrb[:, 0:2, 64:128])
                                        K.cp("act", hq[:, base + 768:base + 1024].re("p (g c) -> p g c", c=128), ptrb[:, 2:4, :])
                                    K.dma("pool", hgD.k(t)[t], hgt)
                                lists = []
                                for bi, t in enumerate(blk):
                                    K.rec_start()
                                    post4(t, bi, pps[bi], SA[bi])
                                    lists.append(K.rec_stop())
                                if bix + 1 < len(blocks):
                                    K.rec_start()
                                    hchain(blocks[bix + 1])
                                    lists.append(K.rec_stop())
                                K.emit_interleaved(lists, lag=0)
                        K.barrier()
                    catBC = K.sb(mix, "catBC", [128, 6, NTOK], BF16)
                    wob = K.sb(mix, "wob", [128, 8, D], BF16)
                    wstg = [K.sb(mix, "wstg%d" % i, [128, 4, D], F32) for i in range(2)]
                    if dbg is not None and dbg[0] == ("A", l):
                        with ExitStack() as ph:
                            d1 = K.sb(ph, "d1", [128, NTOK], F32)
                            d2 = K.sb(ph, "d2", [128, NTOK], BF16)
                            for i in range(4):
                                K.dma("sp", d2, qTD[:, i, :])
                                K.cp("dve", d1, d2)
                                dump(d1, i * 128)
                            K.cp("dve", d1, kT)
                            dump(d1, 512)
                            for i in range(2):
                                K.cp("dve", d1, catA[:, i, :])
                                dump(d1, 640 + i * 128)
                            K.barrier()
                        return nc
                    with ExitStack() as ph:
                        pT = [[K.sb(ph, "pT%d_%d" % (a, i), [128, 512], BF16) for i in range(3)] for a in range(2)]
                        osb = K.sb(ph, "osb", [128, 512], F32)
                        rr = K.sb(ph, "rr", [128, 512], F32)
                        sT = [[K.ps(ph, "sT%d_%d" % (a, i), [128, 512]) for i in range(2)] for a in range(2)]
                        oacc = [K.ps(ph, "oacc%d" % i, [128, 512]) for i in range(2)]
                        Rb = K.ps(ph, "Rb", [128, 512])
                        Vo = K.sb(ph, "Vo", [128, NT, 2, 128], BF16)
                        K.memset("pool", Vo, 0.0)
                        K.memset("pool", Vo[:, :, :, 0:1], 1.0)
                        for kvh_ in range(2):
                            K.cp("pool", Vo[:, :, kvh_, 64:128], Ve[:, :, kvh_, 0:64])
                        for half in range(2):
                            K.dma("sp", wstg[half], w_out[l, half * 512:(half + 1) * 512, :].re("(k p) n -> p k n", p=128))
                            K.cp("pool", wob[:, half * 4:half * 4 + 4, :], wstg[half])
                        qbl = [K.sb(ph, "qbl%d" % i, [128, 4, 512], BF16) for i in range(2)]
                        qblocks = [(0, 256, [0, 1])] + [(256 + 512 * b, 512, list(range(2, NT)) + [0, 1]) for b in range(4)]
                        if l == DEPTH - 1:
                            qblocks = qblocks[1:]
                        si_ = 0
                        for qbi, (tok0, N, stiles) in enumerate(qblocks):
                            qT = qbl[qbi % 2]
                            K.dma("sp", qT[:, :, 0:N], qTD[:, :, tok0:tok0 + N])
                            for j in range(4):
                                even = j % 2 == 0
                                nst = len(stiles)

                                def emit_acc(a, si, s, p_):
                                    if even:
                                        K.mm(oacc[a][0:65, 0:N], Ve[:, s, a, :], p_[:, 0:N], start=(si == 0), stop=(si == nst - 1))
                                    else:
                                        K.mm(oacc[a][:, 0:N], Vo[:, s, a, :], p_[:, 0:N], start=(si == 0), stop=(si == nst - 1))
                                pend = []
                                for si, s in enumerate(stiles):
                                    cur = []
                                    for a in range(2):
                                        st_ = sT[a][si_ % 2]
                                        K.mm(st_[:, 0:N], kT[a * 64:(a + 1) * 64, s * 128:(s + 1) * 128],
                                             qT[a * 64:(a + 1) * 64, j, 0:N])
                                    for a in range(2):
                                        st_ = sT[a][si_ % 2]
                                        p_ = pT[a][si_ % 3]
                                        K.act(p_[:, 0:N], st_[:, 0:N], AF.Exp, scale=0.125)
                                        cur.append((a, si, s, p_))
                                    si_ += 1
                                    pend.append(cur)
                                    if len(pend) > 1:
                                        for args in pend.pop(0):
                                            emit_acc(*args)
                                while pend:
                                    for args in pend.pop(0):
                                        emit_acc(*args)
                                for a in range(2):
                                    head = a * 4 + j
                                    acc = oacc[a]
                                    ch = 2 + head // 2
                                    if even:
                                        K.recip(rr[64:65, 0:N], acc[64:65, 0:N])
                                        K.mm(Rb[0:64, 0:N], onesf[64:65, 0:64], rr[64:65, 0:N])
                                        K.cp("act", osb[0:64, 0:N], acc[0:64, 0:N])
                                        K.tt("dve", catBC.k(("b", head, tok0))[0:64, ch - 2, tok0:tok0 + N], osb[0:64, 0:N],
                                             Rb[0:64, 0:N], ALU.mult)
                                    else:
                                        K.recip(rr[0:1, 0:N], acc[0:1, 0:N])
                                        K.mm(Rb[:, 0:N], onesf[0:1, :], rr[0:1, 0:N])
                                        K.cp("act", osb[64:128, 0:N], acc[64:128, 0:N])
                                        K.tt("dve", catBC.k(("b", head, tok0))[64:128, ch - 2, tok0:tok0 + N], osb[64:128, 0:N],
                                             Rb[64:128, 0:N], ALU.mult)
                        K.barrier()
                if dbg is not None and dbg[0] == ("B", l):
                    with ExitStack() as ph:
                        d1 = K.sb(ph, "d1", [128, NTOK], F32)
                        for i in range(4):
                            K.cp("dve", d1, catBC[:, i, :])
                            dump(d1, i * 128)
                        K.barrier()
                    return nc
                with ExitStack() as ph:
                    hgt2 = [K.sb(ph, "hgt%d" % i, [128, HGC], BF16) for i in range(2)]
                    oF = K.sb(ph, "oF", [128, NT, 256], F32)
                    S = [K.sb(ph, "S%d" % i, [128, 128], F32) for i in range(2)]
                    Sp = [[K.sb(ph, "Sp%d_%d" % (i, c), [128, 128], BF16) for c in range(2)] for i in range(2)]
                    ATm = [K.sb(ph, "ATm%d" % i, [128, 128], BF16) for i in range(4)]
                    osum = K.sb(ph, "osum", [128, 256], F32)
                    y1 = K.sb(ph, "y1", [128, 256], F32)
                    ycb = K.sb(ph, "ycb", [128, 256], BF16)
                    ss = K.sb(ph, "ssC", [128, 4], F32)
                    rs = K.sb(ph, "rsC", [128, 4], F32)
                    UpsB = [K.ps(ph, "Ups%d" % i, [128, 4, 128]) for i in range(2)]
                    ATpB = [K.ps(ph, "ATp%d" % i, [128, 4, 128]) for i in range(2)]
                    ops_ = [K.ps(ph, "ops%d" % i, [128, 512]) for i in range(2)]
                    ptrb = K.ps(ph, "ptrbC", [128, 8, 128], BF16)
                    li = 0
                    for d in range(2):
                        base = d * 1280
                        order = list(range(NT)) if d == 0 else [1, 0] + list(range(NT - 1, 1, -1))
                        corder = (0, 1) if d == 0 else (1, 0)
                        for hp in range(2):
                            K.memset("pool", S[hp], 0.0)
                        for t in order:
                            hgt = hgt2[li % 2]
                            o_ps = ops_[li % 2]
                            li += 1
                            K.dma("sp", hgt, hgD.k(t)[t])
                            tok = slice(t * 128, (t + 1) * 128)
                            SK = os.environ.get("KSKIP", "")
                            for hp in range(2):
                                for c in range(2):
                                    if "U" in SK:
                                        continue
                                    if "c0" in SK and c == 1:
                                        continue
                                    K.mm(UpsB[c][:, hp, :],
                                         hgt[c * 64:(c + 1) * 64, base + 1024 + hp * 128:base + 1024 + (hp + 1) * 128],
                                         hgt[c * 64:(c + 1) * 64, 2560 + hp * 128:2560 + (hp + 1) * 128])
                                for h in range(2):
                                    head = hp * 2 + h
                                    if "AT" in SK:
                                        continue
                                    K.mm(ATpB[h][:, hp, :],
                                         hgt[h * 64:(h + 1) * 64, base + 768 + hp * 128:base + 768 + (hp + 1) * 128],
                                         hgt[h * 64:(h + 1) * 64, base + hp * 128:base + (hp + 1) * 128])
                                    K.cp("act", ATm[head], ATpB[h][:, hp, :])
                                    if "asel" in os.environ.get("KSKIP", ""):
                                        pass
                                    elif d == 0:
                                        K.asel(ATm[head], [[1, 128]], -1, ALU.is_ge)
                                        K.memset("pool", ATm[head][0:64, 64:128], 0.0)
                                    else:
                                        K.asel(ATm[head], [[-1, 128]], 1, ALU.is_ge)
                                        K.memset("pool", ATm[head][64:128, 0:64], 0.0)
                                for c in corder:
                                    ci = 2 * t + c
                                    if "chain" in os.environ.get("KSKIP", ""):
                                        continue
                                    K.ts("dve", Sp[hp][c], S[hp], EH[:, d, hp, ci:ci + 1], None, ALU.mult)
                                    K.stt("dve", S[hp], S[hp], EF[:, d, hp, ci:ci + 1], UpsB[c][:, hp, :], ALU.mult, ALU.add)
                                for h in range(2):
                                    head = hp * 2 + h
                                    if "omm" in SK:
                                        continue
                                    oo = o_ps[:, head * 64:(head + 1) * 64]
                                    K.mm(oo, ATm[head], hgt[:, 2560 + head * 64:2560 + (head + 1) * 64], start=True, stop=False)
                                    if "inter" in os.environ.get("KSKIP", ""):
                                        K.mm(oo, ATm[head], hgt[:, 2560 + head * 64:2560 + (head + 1) * 64], start=False, stop=True)
                                        continue
                                    for ic, c in enumerate(corder):
                                        qz = base + 256 + c * 256 + hp * 128
                                        K.mm(oo, hgt[h * 64:(h + 1) * 64, qz:qz + 128],
                                             Sp[hp][c][h * 64:(h + 1) * 64, h * 64:(h + 1) * 64], start=False, stop=(ic == 1))
                            if "omm" in SK:
                                pass
                            elif d == 0:
                                K.cp("act", oF.k(t)[:, t, :], o_ps[:, 0:256])
                            elif "post" in os.environ.get("KSKIP", ""):
                                K.cp("act", oF.k(t)[:, t, :], o_ps[:, 0:256])
                            else:
                                K.tt("dve", osum, o_ps[:, 0:256], oF.k(t)[:, t, :], ALU.add)
                                K.tt("pool", y1, osum, osum, ALU.mult)
                                K.red("dve", ss, y1.re("p (h c) -> p h c", c=64))
                                K.act(rs, ss, AF.Sqrt, bias=epsc, scale=1.0 / 64)
                                K.recip(rs, rs)
                                K.tt("dve", y1.re("p (h c) -> p h c", c=64), osum.re("p (h c) -> p h c", c=64),
                                     rs.re("p (h o) -> p h o", o=1).tb([128, 4, 64]), ALU.mult)
                                K.tt("pool", y1.re("p (h c) -> p h c", c=64), y1.re("p (h c) -> p h c", c=64),
                                     whb[:, 2:3, :].tb([128, 4, 64]), ALU.mult)
                                K.tt("dve", ycb, y1, hgt[:, 2816:3072], ALU.mult)
                                for g in range(2):
                                    K.tr(ptrb[:, g, :], ycb[:, g * 128:(g + 1) * 128], identb)
                                K.cp("act", catBC.k(("c", t))[:, 4:6, tok], ptrb[:, 0:2, :])
                    K.barrier()
                if dbg is not None and dbg[0] == ("C", l):
                    with ExitStack() as ph:
                        d1 = K.sb(ph, "d1", [128, NTOK], F32)
                        for i in range(2):
                            K.cp("dve", d1, catBC[:, 4 + i, :])
                            dump(d1, i * 128)
                        K.barrier()
                    return nc
                with ExitStack() as ph:
                    G1 = K.sb(ph, "G1", [128, 2, D], F32)
                    tmp = [K.sb(ph, "tmpD%d" % i, [128, 512], F32) for i in range(2)]
                    pso = [K.ps(ph, "pso%d" % i, [128, 512]) for i in range(4)]
                    for ctx in range(2):
                        bload(G1[:, ctx, :], 2, ctx)
                    i = 0
                    for t in tiles:
                        ctx = 1 if t < 2 else 0
                        for half in range(2):
                            pp = pso[i % 4]
                            tm = tmp[i % 2]
                            i += 1
                            for kc in range(8):
                                K.mm(pp, (catA[:, kc, t * 128:(t + 1) * 128] if kc < 2 else catBC[:, kc - 2, t * 128:(t + 1) * 128]), wob[:, kc, half * 512:(half + 1) * 512],
                                     start=(kc == 0), stop=(kc == 7))
                            xv = xs.k(t)[:, t, half * 512:(half + 1) * 512]
                            K.tt("dve", tm, pp, G1[:, ctx, half * 512:(half + 1) * 512], ALU.mult)
                            K.tt("pool", xv, xv, tm, ALU.add)
                    K.barrier()
            if dbg is not None and dbg[0] == ("D", l):
                for t in range(NT):
                    dump(xs[:, t, :], t * 128)
                K.barrier()
                return nc
            etiles = [t for t in tiles if t >= 2] + [t for t in tiles if t < 2]
            with ExitStack() as eph:
                igat = K.sb(eph, "igat", [128, NT, 2], U32)
                gts = K.sb(eph, "gts", [128, NT, 2], F32)
                ring = [K.sb(eph, "ring%d" % i, [128, 4096], F32R) for i in range(4)]
                cnt_ = {"ri": 0, "xi": 0, "yi": 0, "pi": 0}
                wv = {}

                def stageW(e):
                    ri = cnt_["ri"]
                    cnt_["ri"] += 3
                    w1v = ring[ri % 4].re("p (k n) -> p k n", k=8)
                    w3v = ring[(ri + 1) % 4].re("p (k n) -> p k n", k=8)
                    w2v = ring[(ri + 2) % 4].re("p (k n) -> p k n", k=4)
                    K.dma("sp", w1v, w1[l, e].re("(k p) n -> p k n", p=128))
                    K.dma("sp", w3v, w3[l, e].re("(k p) n -> p k n", p=128))
                    K.dma("sp", w2v, w2[l, e].re("(k p) n -> p k n", p=128))
                    wv[e] = (w1v, w3v, w2v)
                with ExitStack() as ph:
                    A2 = K.sb(ph, "A2", [128, D], F32)
                    SH2 = K.sb(ph, "SH2", [128, D], F32)
                    wr = K.sb(ph, "wr", [128, 8, 36], F32)
                    brb = K.sb(ph, "brb", [128, 36], F32)
                    K.dma("sp", wr, w_r[l].re("(k p) n -> p k n", p=128))
                    K.dma("sp", brb, b_r[l:l + 1, :].tb([128, 36]))
                    stageW(0)
                    ecr = K.sb(ph, "ecr", [128, 33], F32)
                    K.dma("sp", ecr, c_ecr)
                    ltm = K.sb(ph, "ltm", [128, 128], F32)
                    K.dma("sp", ltm, c_ltm)
                    baseb = K.sb(ph, "baseb", [128, 32], F32)
                    K.memset("pool", baseb, 0.0)
                    SC = []
                    for q in range(2):
                        sc = {}
                        sc["h2"] = K.sb(ph, "h2_%d" % q, [128, D], F32)
                        sc["h2Tf"] = K.sb(ph, "h2Tf%d" % q, [128, 8, 128], F32)
                        sc["h2h"] = K.sb(ph, "h2h%d" % q, [128, D], BF16)
                        for nm, w in (("lg", 36), ("em", 32), ("sm", 24), ("t8", 8), ("oh", 4), ("s1", 32), ("s2", 32),
                                      ("aa", 32), ("pos", 32), ("vv", 32), ("tq", 32)):
                            sc[nm] = K.sb(ph, "%s%d" % (nm, q), [128, w], F32)
                        sc["isx"] = K.sb(ph, "isc%d" % q, [128, 2], U32)
                        sc["ptr"] = [K.ps(ph, "ptrE%d_%d" % (q, i), [128, 4, 128], F32) for i in range(2)]
                        sc["plg"] = K.ps(ph, "plg%d" % q, [128, 64])
                        sc["pcs"] = K.ps(ph, "pcs%d" % q, [128, 64])
                        SC.append(sc)
                    cur_ctx = [None]

                    def setmod(ctx):
                        if cur_ctx[0] != ctx:
                            cur_ctx[0] = ctx
                            bload(A2, 4, ctx)
                            bload(SH2, 3, ctx)

                    def e1_tile(ti, t, sc):
                        h2 = sc["h2"]; h2Tf = sc["h2Tf"]; lg = sc["lg"]; em = sc["em"]; sm = sc["sm"]; t8 = sc["t8"]
                        oh = sc["oh"]; s1 = sc["s1"]; s2 = sc["s2"]; aa = sc["aa"]; pos = sc["pos"]; vv = sc["vv"]
                        tq = sc["tq"]; isx = sc["isx"]; ptr = sc["ptr"]; plg = sc["plg"]; pcs = sc["pcs"]; h2h = sc["h2h"]
                        setmod(1 if t < 2 else 0)
                        xt = xs.k(t)[:, t, :]
                        K.act(h2, xt, AF.Square, accum=sm[:, 0:1])
                        K.act(sm[:, 1:2], sm[:, 0:1], AF.Sqrt, bias=epsc, scale=1.0 / D)
                        K.recip(sm[:, 1:2], sm[:, 1:2])
                        K.stt("dve", h2, xt, sm[:, 1:2], A2, ALU.mult, ALU.mult)
                        K.tt("pool", h2, h2, SH2, ALU.add)
                        K.cp("pool", h2h, h2)
                        for half in range(2):
                            for j in range(4):
                                kc = half * 4 + j
                                K.tr(ptr[half][:, j, :], h2[:, kc * 128:(kc + 1) * 128], ident)
                            K.cp("act", h2Tf[:, half * 4:half * 4 + 4, :], ptr[half])
                        for kc in range(8):
                            K.mm(plg[:, 0:36], h2Tf[:, kc, :], wr[:, kc, :], start=(kc == 0), stop=(kc == 7))
                        K.tt("dve", lg, plg[:, 0:36], brb, ALU.add)
                        K.red("dve", sm[:, 2:3], lg[:, 0:4], ALU.max)
                        K.ts("dve", oh, lg[:, 0:4], sm[:, 2:3], None, ALU.is_equal)
                        K.ts("dve", sm[:, 3:4], sm[:, 2:3], -1.0, None, ALU.mult)
                        K.act(s1[:, 0:4], lg[:, 0:4], AF.Exp, bias=sm[:, 3:4], accum=sm[:, 4:5])
                        K.recip(sm[:, 5:6], sm[:, 4:5])
                        K.ts("dve", oh, oh, 1e30, -1e30, ALU.mult, ALU.add)
                        K.tt("dve", em.re("p (g c) -> p g c", c=8), lg[:, 4:36].re("p (g c) -> p g c", c=8),
                             oh.re("p (g o) -> p g o", o=1).tb([128, 4, 8]), ALU.add)
                        K.top8(t8, em)
                        K.ts("dve", s1, em, t8[:, 0:1], None, ALU.is_equal)
                        K.ts("dve", s2, em, t8[:, 1:2], None, ALU.is_equal)
                        K.tt("dve", sm[:, 6:7], t8[:, 1:2], t8[:, 0:1], ALU.subtract)
                        K.act(sm[:, 7:8], sm[:, 6:7], AF.Exp)
                        K.ts("dve", sm[:, 7:8], sm[:, 7:8], 1.0, None, ALU.add)
                        K.recip(sm[:, 8:9], sm[:, 7:8])
                        K.ts("dve", sm[:, 9:10], sm[:, 8:9], -1.0, 1.0, ALU.mult, ALU.add)
                        K.tt("dve", sm[:, 8:10], sm[:, 8:10], sm[:, 5:6].tb([128, 2]), ALU.mult)
                        K.tt("dve", aa, s1, s2, ALU.add)
                        K.mm(pcs[:, 0:32], ltm, aa)
                        K.mm(pcs[:, 32:64], onesf, aa)
                        K.tt("dve", pos, pcs[:, 0:32], baseb, ALU.add)
                        K.tt("dve", baseb, pcs[:, 32:64], baseb, ALU.add)
                        K.tt("pool", vv, pos, ecr[:, 0:32], ALU.add)
                        for k, sk in enumerate((s1, s2)):
                            c0 = 10 + 6 * k
                            K.tt("dve", tq, pos, sk, ALU.mult)
                            K.red("dve", sm[:, c0:c0 + 1], tq)
                            K.tt("dve", tq, vv, sk, ALU.mult)
                            K.red("dve", sm[:, c0 + 1:c0 + 2], tq)
                            K.ts("dve", sm[:, c0 + 2:c0 + 3], sm[:, c0:c0 + 1], float(CAP), None, ALU.is_lt)
                            K.ts("dve", sm[:, c0 + 3:c0 + 4], ecr[:, 32:33], float(NE * CAP + (ti * 2 + k) * 128), None, ALU.add)
                            K.tt("dve", sm[:, c0 + 4:c0 + 5], sm[:, c0 + 1:c0 + 2], sm[:, c0 + 3:c0 + 4], ALU.subtract)
                            K.stt("dve", sm[:, c0 + 4:c0 + 5], sm[:, c0 + 4:c0 + 5], sm[:, c0 + 2:c0 + 3], sm[:, c0 + 3:c0 + 4],
                                  ALU.mult, ALU.add)
                            K.cp("dve", isx[:, k:k + 1], sm[:, c0 + 4:c0 + 5])
                            K.tt("dve", sm[:, c0 + 5:c0 + 6], sm[:, c0 + 1:c0 + 2], sm[:, c0 + 2:c0 + 3], ALU.mult)
                            K.cp("dve", igat.k(t)[:, t, k:k + 1], sm[:, c0 + 5:c0 + 6])
                            K.tt("dve", gts.k(t)[:, t, k:k + 1], sm[:, 8 + k:9 + k], sm[:, c0 + 2:c0 + 3], ALU.mult)
                            K.op("pool", (lambda e, isx=isx, k=k, h2h=h2h: e.indirect_dma_start(
                                out=XS.ap, out_offset=bass.IndirectOffsetOnAxis(isx.ap[:, k:k + 1], 0),
                                in_=h2h.ap, in_offset=None)), [h2h, isx], [XS.k(("s", ti, k))], dma=True)
                    for p_ in range(0, len(etiles), 2):
                        lists = []
                        for q in range(2):
                            if p_ + q < len(etiles):
                                K.rec_start()
                                e1_tile(p_ + q, etiles[p_ + q], SC[q])
                                lists.append(K.rec_stop())
                        K.emit_interleaved(lists)
                    K.barrier()
                with ExitStack() as ph:
                    XTb = [K.sb(ph, "XT%d" % i, [128, 8, CAP], F32R) for i in range(2)]
                    hTgb = [K.sb(ph, "hTg%d" % i, [128, 4, CAP], F32R) for i in range(2)]
                    xe = [K.sb(ph, "xe%d" % i, [128, D], BF16) for i in range(3)]
                    yst = [K.sb(ph, "yst%d" % i, [128, D], BF16) for i in range(3)]
                    sa = [K.sb(ph, "sa%d" % i, [128, CAP], F32) for i in range(2)]
                    ptr = [K.ps(ph, "ptrX%d" % i, [128, 4, 128], BF16) for i in range(2)]
                    pab = [K.ps(ph, "pab%d" % i, [128, 512]) for i in range(4)]
                    py = [K.ps(ph, "py%d" % i, [128, 512]) for i in range(2)]
                    if os.environ.get("KVERB"):
                        print("phase E2 sbuf remaining", nc.sbuf_bytes_remaining, flush=True)
                    NCT = CAP // 128
                    def stageX(e):
                        XT = XTb[e % 2]
                        for ct in range(NCT):
                            x_ = xe[cnt_["xi"] % 3]
                            cnt_["xi"] += 1
                            K.dma("sp", x_, XS[e * CAP + ct * 128:e * CAP + (ct + 1) * 128, :])
                            for half in range(2):
                                for j in range(4):
                                    kc = half * 4 + j
                                    K.tr(ptr[half][:, j, :], x_[:, kc * 128:(kc + 1) * 128], identb)
                                K.cp("act" if half == 0 else "dve", XT[:, half * 4:half * 4 + 4, ct * 128:(ct + 1) * 128], ptr[half])

                    def stageAB(e):
                        XT = XTb[e % 2]
                        hTg = hTgb[e % 2]
                        w1v, w3v, w2v = wv[e]
                        for ffc in range(4):
                            i = cnt_["pi"]
                            cnt_["pi"] += 2
                            pa = pab[i % 4]
                            pb = pab[(i + 1) % 4]
                            sa_ = sa[(i // 2) % 2]
                            for kc in range(8):
                                K.mm(pa[:, 0:CAP], w1v[:, kc, ffc * 128:(ffc + 1) * 128], XT[:, kc, :],
                                     start=(kc == 0), stop=(kc == 7))
                            for kc in range(8):
                                K.mm(pb[:, 0:CAP], w3v[:, kc, ffc * 128:(ffc + 1) * 128], XT[:, kc, :],
                                     start=(kc == 0), stop=(kc == 7))
                            K.act(sa_, pa[:, 0:CAP], AF.Silu)
                            K.tt("dve", hTg[:, ffc, :], sa_, pb[:, 0:CAP], ALU.mult)

                    def stageY(e):
                        hTg = hTgb[e % 2]
                        w1v, w3v, w2v = wv[e]
                        for ct in range(NCT):
                            y_ = yst[cnt_["yi"] % 3]
                            cnt_["yi"] += 1
                            for half in range(2):
                                pyy = py[half]
                                for ffc in range(4):
                                    K.mm(pyy, hTg[:, ffc, ct * 128:(ct + 1) * 128], w2v[:, ffc, half * 512:(half + 1) * 512],
                                         start=(ffc == 0), stop=(ffc == 3))
                                K.cp("act" if half == 0 else "dve", y_[:, half * 512:(half + 1) * 512], pyy)
                            K.dma("pool", YS.k((e, ct))[e * CAP + ct * 128:e * CAP + (ct + 1) * 128, :], y_)

                    if "noE2" not in os.environ.get("KSKIP", ""):
                        stageX(0)
                        for e in range(NE):
                            stageAB(e)
                            if e + 1 < NE:
                                stageX(e + 1)
                                stageW(e + 1)
                            stageY(e)
                    K.barrier()
                with ExitStack() as ph:
                    G2m = [K.sb(ph, "G2_%d" % i, [128, D], F32) for i in range(2)]
                    for ctx in range(2):
                        bload(G2m[ctx], 5, ctx)
                    yg = [[K.sb(ph, "yg%d_%d" % (i, k), [128, D], BF16) for k in range(2)] for i in range(3)]
                    mt = K.sb(ph, "mt", [128, D], F32)
                    for ti, t in enumerate(etiles):
                        if "noE3" in os.environ.get("KSKIP", ""):
                            continue
                        ctx = 1 if t < 2 else 0
                        ya_, yb_ = yg[ti % 3]
                        for k, yy in enumerate((ya_, yb_)):
                            K.op("pool", (lambda e, yy=yy, t=t, k=k: e.indirect_dma_start(
                                out=yy.ap, out_offset=None, in_=YS.ap,
                                in_offset=bass.IndirectOffsetOnAxis(igat.ap[:, t, k:k + 1], 0))), [YS, igat], [yy], dma=True)
                        K.ts("dve", mt, ya_, gts[:, t, 0:1], None, ALU.mult)
                        K.stt("dve", mt, yb_, gts[:, t, 1:2], mt, ALU.mult, ALU.add)
                        K.tt("dve", mt, mt, G2m[ctx], ALU.mult)
                        xv = xs.k(t)[:, t, :]
                        K.tt("dve", xv, xv, mt, ALU.add)
                    K.barrier()
            if dbg is not None and dbg[0] == ("E", l):
                for t in range(NT):
                    dump(xs[:, t, :], t * 128)
                K.barrier()
                return nc
        for t in range(2, NT):
            K.dma("sp", out[(t - 2) * 128:(t - 1) * 128, :], xs.k(t)[:, t, :])
        K.barrier()
    return nc


def make_in_maps(inputs, n_cores=8):
    f = lambda a: np.ascontiguousarray(np.asarray(a, dtype=np.float32))
    x = f(inputs["x"]); c = f(inputs["c"]); ctx = f(inputs["ctx"]); c_ctx = f(inputs["c_ctx"])
    consts = host_consts()
    shared = {
        "w_mod": f(inputs["w_mod"]), "b_mod": f(inputs["b_mod"]),
        "norm1_w": f(inputs["norm1_w"]), "norm2_w": f(inputs["norm2_w"]),
        "w_in": f(inputs["w_in"]), "w_s": f(inputs["w_s"]),
        "b_sT": f(np.transpose(inputs["b_s"], (0, 2, 1))),
        "qkh_w": f(np.stack([inputs["q_norm_w"], inputs["k_norm_w"], inputs["hgrn_norm_w"]], axis=1)),
        "lbl": f(np.asarray(inputs["hgrn_lb_logits"]).reshape(DEPTH, 512)),
        "w_out": f(inputs["w_out"]),
        "w_r": f(np.concatenate([inputs["w_grp"], inputs["w_exp"]], axis=2)),
        "b_r": f(np.concatenate([inputs["b_grp"], inputs["b_exp"]], axis=1)),
        "w1": f(inputs["w1"]), "w3": f(inputs["w3"]), "w2": f(inputs["w2"]),
    }
    shared.update(consts)
    maps = []
    for b in range(n_cores):
        m = dict(shared)
        m["xin"] = np.ascontiguousarray(np.concatenate([ctx[b], x[b]], axis=0))
        cc = np.stack([c[b], c_ctx], axis=1)
        m["ccT"] = np.ascontiguousarray(cc.reshape(8, 128, 2).transpose(1, 0, 2))
        maps.append(m)
    return maps


def kernel(**inputs):
    nc = bass.Bass("TRN2", target_bir_lowering=False)
    build(nc)
    maps = make_in_maps(inputs, 8)
    res = run_bass_kernel_spmd(nc, maps, core_ids=list(range(8)))
    return np.stack([np.asarray(r["out"], dtype=np.float32) for r in res.results], axis=0)
```

```python
import os
import numpy as np
from contextlib import ExitStack
import ml_dtypes
import concourse.bass as bass
import concourse.mybir as mybir
from concourse.bass_utils import run_bass_kernel_spmd

F32 = mybir.dt.float32
F32R = mybir.dt.float32r
BF16 = mybir.dt.bfloat16
AF = mybir.ActivationFunctionType
ALU = mybir.AluOpType
AX = mybir.AxisListType

D = 1024
NT = 18
NTOK = NT * 128
DEPTH = 2
INW = 2560
EPS = 1e-6
NE = 32
DFF = 512
HGC = 3072
CAP = 512
U32 = mybir.dt.uint32


class Buf:
    __slots__ = ("name", "st")

    def __init__(self, name):
        self.name = name
        self.st = {}


class V:
    __slots__ = ("buf", "key", "ap")

    def __init__(self, buf, key, ap):
        self.buf = buf
        self.key = key
        self.ap = ap

    def __getitem__(self, i):
        return V(self.buf, self.key, self.ap[i])

    def k(self, key):
        return V(self.buf, key, self.ap)

    def bc(self, dt):
        return V(self.buf, self.key, self.ap.bitcast(dt))

    def re(self, pat, **kw):
        return V(self.buf, self.key, self.ap.rearrange(pat, **kw))

    def tb(self, shape):
        return V(self.buf, self.key, self.ap.to_broadcast(shape))


class Op:
    __slots__ = ("eng", "n", "sem", "val", "ep")


class Kern:
    ENG = ["pe", "act", "dve", "pool", "sp"]

    def __init__(self, nc):
        self.nc = nc
        self.e = {"pe": nc.tensor, "act": nc.scalar, "dve": nc.vector, "pool": nc.gpsimd, "sp": nc.sync}
        self.sem = {n: nc.alloc_semaphore("es_" + n) for n in self.ENG}
        self.cnt = {n: 0 for n in self.ENG}
        self.waited = {n: {} for n in self.ENG}
        self.dq = {"sp": [nc.alloc_semaphore("dsp%d" % i) for i in range(40)],
                   "pool": [nc.alloc_semaphore("dpl%d" % i) for i in range(12)],
                   "act": [nc.alloc_semaphore("dac%d" % i) for i in range(8)]}
        self.dqi = {"sp": 0, "pool": 0, "act": 0}
        self.dqv = {}
        self.nops = 0
        self.g1 = nc.alloc_semaphore("gate1")
        self.g2 = nc.alloc_semaphore("gate2")
        self.nreset = 0
        self.noinc_ok = True
        self.uid = 0
        self.pending = None
        self._rec = None

    def sb(self, es, name, shape, dt):
        self.uid += 1
        name = "%s_u%d" % (name, self.uid)
        h = es.enter_context(self.nc.sbuf_tensor(name, list(shape), dt))
        return V(Buf(name), None, h[:])

    def ps(self, es, name, shape, dt=F32):
        self.uid += 1
        name = "%s_u%d" % (name, self.uid)
        h = es.enter_context(self.nc.psum_tensor(name, list(shape), dt))
        return V(Buf(name), None, h[:])

    def dram(self, name, shape, dt, kind="Internal"):
        h = self.nc.dram_tensor(name, list(shape), dt, kind=kind)
        return V(Buf(name), None, h.ap())

    @staticmethod
    def _ents(v):
        st = v.buf.st
        if v.key is None:
            return list(st.values())
        r = []
        if v.key in st:
            r.append(st[v.key])
        if None in st:
            r.append(st[None])
        return r

    def _collect(self, reads, writes):
        deps = []
        for v in reads:
            for ent in self._ents(v):
                if ent[0] is not None:
                    deps.append(ent[0])
        for v in writes:
            for ent in self._ents(v):
                if ent[0] is not None:
                    deps.append(ent[0])
                deps.extend(ent[1].values())
        return deps

    def _register(self, op, reads, writes):
        for v in reads:
            st = v.buf.st
            if v.key not in st:
                st[v.key] = [None, {}]
            rk = op.eng if op.sem is None else ("d", id(op.sem))
            st[v.key][1][rk] = op
        for v in writes:
            st = v.buf.st
            if v.key is None:
                st.clear()
            st[v.key] = [op, {}]

    def _wait(self, eng, sem, val):
        w = self.waited[eng]
        k = id(sem)
        if w.get(k, 0) >= val:
            return
        w[k] = val
        if self.pending is not None:
            self.pending.append((sem, val))
        else:
            self.e[eng].wait_ge(sem, val)

    def rec_start(self):
        self._rec = []

    def rec_stop(self):
        r = self._rec
        self._rec = None
        return r

    def emit_interleaved(self, lists, lag=4):
        if len(lists) > 2:
            ls = [list(x) for x in lists if x]
            while ls:
                for x in list(ls):
                    o_ = x.pop(0)
                    self.op(*o_[0], **o_[1])
                    if not x:
                        ls.remove(x)
            return
        a = lists[0]
        b = lists[1] if len(lists) > 1 else []
        i = j = 0
        while i < len(a) or j < len(b):
            if i < len(a):
                self.op(*a[i][0], **a[i][1])
                i += 1
            if j < len(b) and (i - j > lag or i >= len(a)):
                self.op(*b[j][0], **b[j][1])
                j += 1

    def op(self, eng, fn, reads=(), writes=(), dma=False, noinc=False, multi=False):
        if self._rec is not None:
            self._rec.append(((eng, fn, reads, writes), dict(dma=dma, noinc=noinc, multi=multi)))
            return None
        reads = [v for v in reads if isinstance(v, V)]
        writes = [v for v in writes if isinstance(v, V)]
        deps = self._collect(reads, writes)
        attach = not (dma or multi or os.environ.get("KNOATTACH"))
        if attach:
            self.pending = []
        for d in deps:
            if d.ep != self.nreset:
                continue
            if d.sem is not None:
                self._wait(eng, d.sem, d.val)
            else:
                if d.eng == "pe" and eng == "pe" and not dma:
                    continue
                self._wait(eng, self.sem[d.eng], d.n)
        o = Op()
        o.ep = self.nreset
        o.eng = eng
        o.sem = None
        o.val = 0
        o.n = 0
        if dma:
            q = self.dq[eng]
            i = self.dqi[eng]
            self.dqi[eng] = (i + 1) % len(q)
            s = q[i]
            prev = self.dqv.get(id(s), 0)
            if prev:
                self._wait(eng, s, prev)
            ins = fn(self.e[eng])
            ins.then_inc(s, 16)
            self.dqv[id(s)] = prev + 16
            o.sem = s
            o.val = prev + 16
        else:
            last = None
            if attach:
                pend = self.pending
                self.pending = None
                for (sm_, vl_) in pend[:-1]:
                    self.e[eng].wait_ge(sm_, vl_)
                if pend:
                    last = pend[-1]
            ins = fn(self.e[eng])
            if last is not None:
                ins._wait_ge(last[0], last[1])
            if noinc:
                o.n = self.cnt[eng] + 1
            else:
                self.cnt[eng] += 1
                ins.then_inc(self.sem[eng], 1)
                o.n = self.cnt[eng]
        self.nops += 1
        self._register(o, reads, writes)
        return o

    def barrier(self, reset=False):
        for eng in self.ENG:
            for other in self.ENG:
                if other != eng and self.cnt[other]:
                    self._wait(eng, self.sem[other], self.cnt[other])
            for q in self.dq.values():
                for s in q:
                    v = self.dqv.get(id(s), 0)
                    if v:
                        self._wait(eng, s, v)
        if not reset:
            return
        self.nreset += 1
        for eng in self.ENG:
            self.e[eng].sem_inc(self.g1, 1)
        pool = self.e["pool"]
        pool.wait_ge(self.g1, 5 * self.nreset)
        for n in self.ENG:
            pool.sem_clear(self.sem[n])
        for q in self.dq.values():
            for s in q:
                if self.dqv.get(id(s), 0):
                    pool.sem_clear(s)
        pool.sem_inc(self.g2, 1)
        for eng in self.ENG:
            self.e[eng].wait_ge(self.g2, self.nreset)
        self.cnt = {n: 0 for n in self.ENG}
        self.waited = {n: {} for n in self.ENG}
        self.dqv = {}

    def mm(self, out, lhsT, rhs, start=True, stop=True):
        rd = [lhsT, rhs] + ([] if start else [out])
        return self.op("pe", lambda e: e.matmul(out.ap, lhsT.ap, rhs.ap, start=start, stop=stop), rd, [out],
                       noinc=(not stop) and self.noinc_ok and not os.environ.get('KNONOINC'))

    def tr(self, out, in_, ident):
        return self.op("pe", lambda e: e.transpose(out.ap, in_.ap, ident.ap), [in_, ident], [out])

    def act(self, out, in_, func, bias=0.0, scale=1.0, accum=None):
        rd = [in_, bias, scale]
        wr = [out] + ([accum] if accum is not None else [])
        b = bias.ap if isinstance(bias, V) else bias
        sc = scale.ap if isinstance(scale, V) else scale
        if accum is None:
            return self.op("act", lambda e: e.activation(out=out.ap, in_=in_.ap, func=func, bias=b, scale=sc), rd, wr)
        return self.op("act", lambda e: e.activation(out=out.ap, in_=in_.ap, func=func, bias=b, scale=sc,
                                                     accum_out=accum.ap), rd, wr, multi=True)

    def tt(self, eng, out, a, b, op):
        if eng == "pool" and not os.environ.get("KPOOLTT"):
            eng = "dve"
        return self.op(eng, lambda e: e.tensor_tensor(out=out.ap, in0=a.ap, in1=b.ap, op=op), [a, b], [out])

    def ts(self, eng, out, a, s1, s2, op0, op1=None):
        x1 = s1.ap if isinstance(s1, V) else s1
        x2 = s2.ap if isinstance(s2, V) else s2
        if op1 is None:
            return self.op(eng, lambda e: e.tensor_scalar(out=out.ap, in0=a.ap, scalar1=x1, scalar2=None, op0=op0),
                           [a, s1], [out])
        return self.op(eng, lambda e: e.tensor_scalar(out=out.ap, in0=a.ap, scalar1=x1, scalar2=x2, op0=op0, op1=op1),
                       [a, s1, s2], [out])

    def stt(self, eng, out, a, s, b, op0, op1):
        x = s.ap if isinstance(s, V) else s
        return self.op(eng, lambda e: e.scalar_tensor_tensor(out=out.ap, in0=a.ap, scalar=x, in1=b.ap, op0=op0, op1=op1),
                       [a, s, b], [out])

    def cp(self, eng, out, in_):
        if eng == "act":
            return self.op("act", lambda e: e.copy(out=out.ap, in_=in_.ap), [in_], [out])
        return self.op(eng, lambda e: e.tensor_copy(out=out.ap, in_=in_.ap), [in_], [out])

    def red(self, eng, out, in_, op=ALU.add):
        return self.op(eng, lambda e: e.tensor_reduce(out=out.ap, in_=in_.ap, axis=AX.X, op=op), [in_], [out])

    def rstd(self, out, in_, scale, nhalf):
        self.ts("dve", out, in_, scale, EPS, ALU.mult, ALU.add)
        return self.op("pool", lambda e: e.tensor_tensor(out=out.ap, in0=out.ap, in1=nhalf.ap, op=ALU.pow),
                       [out, nhalf], [out])

    def recip(self, out, in_):
        return self.op("dve", lambda e: e.reciprocal(out=out.ap, in_=in_.ap), [in_], [out])

    def memset(self, eng, out, val):
        return self.op(eng, lambda e: e.memset(out.ap, val), [], [out])

    def dma(self, q, out, in_):
        return self.op(q, lambda e: e.dma_start(out=out.ap, in_=in_.ap), [in_], [out], dma=True)

    def asel(self, out, pattern, cm, cmp):
        return self.op("pool", lambda e: e.affine_select(out=out.ap, in_=out.ap, pattern=pattern, compare_op=cmp,
                                                         fill=0.0, base=0, channel_multiplier=cm), [out], [out], multi=True)

    def top8(self, out, in_):
        return self.op("dve", lambda e: e.max(out=out.ap, in_=in_.ap), [in_], [out])


def host_consts():
    c = {}
    c["ident"] = np.eye(128, dtype=np.float32)
    c["identb"] = np.eye(128, dtype=np.float32).astype(ml_dtypes.bfloat16)
    s = np.arange(128)[:, None]
    t = np.arange(128)[None, :]
    same = (s // 64) == (t // 64)
    TF = (same & (s <= t)).astype(np.float32)
    TB = (same & (s >= t)).astype(np.float32)
    BO = same.astype(np.float32)
    c["hmat"] = np.stack([TF - 0.5 * BO, BO - TF, TB - 0.5 * BO, BO - TB]).transpose(1, 0, 2).copy()
    ind = np.zeros((128, 2), np.float32)
    ind[:64, 0] = 1
    ind[64:, 1] = 1
    c["ind"] = ind
    ecr = np.zeros((128, 33), np.float32)
    ecr[:, :32] = (np.arange(32) * CAP)[None, :]
    ecr[:, 32] = np.arange(128)
    c["ecr"] = ecr
    c["ltm"] = (s < t).astype(np.float32)
    rows = 2048 // 64
    row = np.repeat(np.arange(rows), 64).astype(np.float32)
    col = np.tile(np.arange(64), rows).astype(np.float32)
    inv = (1.0 / (10000.0 ** (np.arange(0, 32, 2, dtype=np.float32) / 32.0))).astype(np.float32)
    ang = np.stack([row[:, None] * inv, col[:, None] * inv], axis=1).astype(np.float32)
    cos = np.concatenate([np.ones((256, 2, 16), np.float32), np.cos(ang)], 0).reshape(NT, 128, 32)
    sin = np.concatenate([np.zeros((256, 2, 16), np.float32), np.sin(ang)], 0).reshape(NT, 128, 32)
    c["rope"] = np.stack([cos, sin], 2).transpose(1, 0, 2, 3).reshape(128, NT * 64).astype(np.float32).copy()
    return c


def build(nc, n_layers=DEPTH, dbg=None):
    nc.dge_precook = False
    K = Kern(nc)
    xin = K.dram("xin", [NTOK, D], F32, "ExternalInput")
    ccT = K.dram("ccT", [128, 8, 2], F32, "ExternalInput")
    w_mod = K.dram("w_mod", [DEPTH, D, 6 * D], F32R, "ExternalInput")
    b_mod = K.dram("b_mod", [DEPTH, 6 * D], F32, "ExternalInput")
    norm1_w = K.dram("norm1_w", [DEPTH, D], F32, "ExternalInput")
    norm2_w = K.dram("norm2_w", [DEPTH, D], F32, "ExternalInput")
    w_in = K.dram("w_in", [DEPTH, D, INW], F32R, "ExternalInput")
    w_s = K.dram("w_s", [DEPTH, 4, 128, 128], F32, "ExternalInput")
    b_sT = K.dram("b_sT", [DEPTH, 128, 4], F32, "ExternalInput")
    qkh_w = K.dram("qkh_w", [DEPTH, 3, 64], F32, "ExternalInput")
    lbl = K.dram("lbl", [DEPTH, 512], F32, "ExternalInput")
    w_out = K.dram("w_out", [DEPTH, D, D], F32, "ExternalInput")
    w_r = K.dram("w_r", [DEPTH, D, 36], F32, "ExternalInput")
    b_r = K.dram("b_r", [DEPTH, 36], F32, "ExternalInput")
    w1 = K.dram("w1", [DEPTH, NE, D, DFF], F32R, "ExternalInput")
    w3 = K.dram("w3", [DEPTH, NE, D, DFF], F32R, "ExternalInput")
    w2 = K.dram("w2", [DEPTH, NE, DFF, D], F32R, "ExternalInput")
    c_ident = K.dram("ident", [128, 128], F32, "ExternalInput")
    c_identb = K.dram("identb", [128, 128], BF16, "ExternalInput")
    c_hmat = K.dram("hmat", [128, 4, 128], F32, "ExternalInput")
    c_ind = K.dram("ind", [128, 2], F32, "ExternalInput")
    c_rope = K.dram("rope", [128, NT * 64], F32, "ExternalInput")
    out = K.dram("out", [2048, D], F32, "ExternalOutput")
    modD = K.dram("modD", [2, 6 * D], F32)
    hgD = K.dram("hgD", [NT, 128, HGC], BF16)
    qTD = K.dram("qTD", [128, 4, NTOK], BF16)
    XS = K.dram("XS", [NE * CAP + 2 * NTOK, D], BF16)
    YS = K.dram("YS", [NE * CAP, D], BF16)
    c_ecr = K.dram("ecr", [128, 33], F32, "ExternalInput")
    c_ltm = K.dram("ltm", [128, 128], F32, "ExternalInput")
    dbgD = None
    if dbg is not None:
        dbgD = K.dram("dbg", list(dbg[1]), F32, "ExternalOutput")

    with ExitStack() as top:
        xs = K.sb(top, "xs", [128, NT, D], F32)
        ident = K.sb(top, "ident_s", [128, 128], F32)
        identb = K.sb(top, "identb_s", [128, 128], BF16)
        hmat = K.sb(top, "hmat_s", [128, 4, 128], F32)
        ind = K.sb(top, "ind_s", [128, 2], F32)
        onesf = K.sb(top, "onesf", [128, 128], F32)
        epsc = K.sb(top, "epsc", [128, 1], F32)
        K.dma("sp", ident, c_ident)
        K.dma("sp", identb, c_identb)
        K.dma("sp", hmat, c_hmat)
        K.dma("sp", ind, c_ind)
        K.memset("pool", onesf, 1.0)
        K.memset("pool", epsc, EPS)
        nhalf = K.sb(top, "nhalf", [128, 8], F32)
        K.memset("pool", nhalf, -0.5)
        for t in range(NT):
            K.dma("sp", xs.k(t)[:, t, :], xin[t * 128:(t + 1) * 128, :])

        def dump(v2d, r0=0):
            K.dma("pool", dbgD[r0:r0 + v2d.ap.shape[0], 0:v2d.ap.shape[1]], v2d)

        for l in range(n_layers):
            tiles = list(range(NT)) if l < DEPTH - 1 else list(range(2, NT))
            K.noinc_ok = False
            with ExitStack() as ph:
                cs = K.sb(ph, "cs", [128, 8, 2], F32)
                csr = K.sb(ph, "csr", [128, 8, 2], F32R)
                wm = [K.sb(ph, "wm%d" % i, [128, 3072], F32R) for i in range(2)]
                mrow = K.sb(ph, "mrow", [2, 6 * D], F32)
                brow = K.sb(ph, "brow", [2, 6 * D], F32)
                nrow = K.sb(ph, "nrow", [2, 2, D], F32)
                mps = [K.ps(ph, "mps%d" % i, [2, 512]) for i in range(6)]
                K.dma("sp", cs, ccT)
                K.act(csr, cs, AF.Silu)
                K.dma("sp", brow, b_mod[l:l + 1, :].tb([2, 6 * D]))
                K.dma("sp", nrow[:, 0, :], norm1_w[l:l + 1, :].tb([2, D]))
                K.dma("sp", nrow[:, 1, :], norm2_w[l:l + 1, :].tb([2, D]))
                i = 0
                for half in range(2):
                    for kc in range(8):
                        w = wm[i % 2]
                        i += 1
                        K.dma("sp", w, w_mod[l, kc * 128:(kc + 1) * 128, half * 3072:(half + 1) * 3072])
                        for n in range(6):
                            K.mm(mps[n], csr[:, kc, :], w[:, n * 512:(n + 1) * 512], start=(kc == 0), stop=(kc == 7))
                    for n in range(6):
                        c0 = half * 3072 + n * 512
                        K.tt("dve", mrow[:, c0:c0 + 512], mps[n], brow[:, c0:c0 + 512], ALU.add)
                K.stt("dve", mrow[:, D:2 * D], mrow[:, D:2 * D], 1.0, nrow[:, 0, :], ALU.add, ALU.mult)
                K.stt("dve", mrow[:, 4 * D:5 * D], mrow[:, 4 * D:5 * D], 1.0, nrow[:, 1, :], ALU.add, ALU.mult)
                K.dma("sp", modD, mrow)
                K.barrier()

            K.noinc_ok = True

            def bload(dst, slot, ctx):
                K.dma("sp", dst, modD[ctx:ctx + 1, slot * D:(slot + 1) * D].tb([128, D]))

            with ExitStack() as mix:
                catA = K.sb(mix, "catA", [128, 2, NTOK], BF16)
                EF = K.sb(mix, "EF", [128, 2, 2, 2 * NT], F32)
                EH = K.sb(mix, "EH", [128, 2, 2, 2 * NT], F32)
                whb = K.sb(mix, "whb", [128, 3, 64], F32)
                K.dma("sp", whb.re("p a c -> p (a c)"), qkh_w[l:l + 1].re("o a c -> o (a c)").tb([128, 192]))
                with ExitStack() as ab:
                    kT = K.sb(mix, "kT", [128, NTOK], BF16)
                    Ve = K.sb(mix, "Ve", [128, NT, 2, 65], BF16)
                    with ExitStack() as ph:
                        A1 = K.sb(ph, "A1", [128, D], F32)
                        SH1 = K.sb(ph, "SH1", [128, D], F32)
                        rope = K.sb(ph, "rope_s", [128, NT, 2, 32], F32)
                        K.dma("sp", rope.re("p t a c -> p (t a c)"), c_rope)
                        wsn = K.sb(ph, "wsn", [128, 4, 128], F32)
                        wsT = K.sb(ph, "wsT", [128, 4, 128], F32R)
                        bsT = K.sb(ph, "bsT", [128, 4], F32)
                        lbB = K.sb(ph, "lbB", [128, 512], F32)
                        omlB = K.sb(ph, "omlB", [128, 512], F32)
                        hTb = K.sb(ph, "hTb", [128, 8, 256], F32R)
                        wc = [K.sb(ph, "wc%d" % i, [128, 8, 256], F32R) for i in range(2)]
                        hbuf = K.sb(ph, "hbuf", [128, D], F32)
                        t2 = K.sb(ph, "t2", [128, 512], F32)
                        SA = []
                        for q_ in range(2):
                            sc = {}
                            sc["zb"] = K.sb(ph, "zb%d" % q_, [128, 512], F32)
                            sc["t1"] = K.sb(ph, "t1_%d" % q_, [128, 512], F32)
                            sc["t3"] = K.sb(ph, "t3_%d" % q_, [128, 512], F32)
                            sc["vnr"] = K.sb(ph, "vnr%d" % q_, [128, 256], F32R)
                            sc["yab"] = K.sb(ph, "yab%d" % q_, [128, 256], BF16)
                            sc["qrb"] = K.sb(ph, "qrb%d" % q_, [128, 512], BF16)
                            sc["ss"] = K.sb(ph, "ss%d" % q_, [128, 8], F32)
                            sc["rs"] = K.sb(ph, "rs%d" % q_, [128, 8], F32)
                            sc["qst"] = K.sb(ph, "qst%d" % q_, [128, 4, 128], BF16)
                            sc["ptrb"] = K.ps(ph, "ptrb%d" % q_, [128, 4, 128], BF16)
                            sc["hg"] = K.sb(ph, "hgs%d" % q_, [128, HGC], BF16)
                            sc["kkb"] = K.sb(ph, "kkb%d" % q_, [128, 512], F32)
                            sc["t2"] = K.sb(ph, "t2_%d" % q_, [128, 512], F32)
                            sc["qeb"] = K.sb(ph, "qeb%d" % q_, [128, 256], BF16)
                            sc["keb"] = K.sb(ph, "keb%d" % q_, [128, 256], BF16)
                            sc["pd"] = K.ps(ph, "pd%d" % q_, [128, 512])
                            K.memset("pool", sc["hg"], 0.0)
                            SA.append(sc)
                        zb = SA[0]["zb"]; t1 = SA[0]["t1"]; t3 = SA[0]["t3"]; ss = SA[0]["ss"]; rs = SA[0]["rs"]
                        ptrb = SA[0]["ptrb"]
                        sqh = [K.sb(ph, "sqh%d" % i, [128, 256], F32) for i in range(2)]
                        logf = [K.sb(ph, "logf%d" % i, [128, 512], F32) for i in range(2)]
                        ppA = [K.ps(ph, "ppA%d" % i, [128, 512]) for i in range(2)]
                        ptr = K.ps(ph, "ptr", [128, 4, 128], F32)
                        pmx = K.ps(ph, "pmx", [128, 512])
                        if os.environ.get("KVERB"):
                            print("phase A sbuf remaining", nc.sbuf_bytes_remaining, flush=True)
                        K.memset("pool", Ve[:, :, :, 64:65], 1.0)
                        K.dma("sp", wsn, w_s[l].re("h t s -> t h s"))
                        K.dma("sp", bsT, b_sT[l])
                        for h in range(4):
                            K.tr(ptr[:, h, :], wsn[:, h, :], ident)
                        K.cp("act", wsT, ptr)
                        if l == 0:
                            K.memset("pool", lbB, 0.0)
                            K.memset("pool", omlB, 1.0)
                        else:
                            K.dma("sp", t1, lbl[1:2, :].tb([128, 512]))
                            K.dma("sp", t2, lbl[0:1, :].tb([128, 512]))
                            K.tt("dve", t1, t1, t2, ALU.subtract)
                            K.act(lbB, t1, AF.Sigmoid)
                            K.ts("dve", omlB, lbB, -1.0, 1.0, ALU.mult, ALU.add)

                        def headnorm(src, nh, w_idx, dst, S=None):
                            S = S or SA[0]
                            t3 = S["t3"]; ss = S["ss"]; rs = S["rs"]
                            K.tt("pool", t3[:, 0:nh * 64], src, src, ALU.mult)
                            K.red("dve", ss[:, 0:nh], t3[:, 0:nh * 64].re("p (h c) -> p h c", c=64))
                            K.rstd(rs[:, 0:nh], ss[:, 0:nh], 1.0 / 64, nhalf[:, 0:nh])
                            K.tt("dve", dst.re("p (h c) -> p h c", c=64), src.re("p (h c) -> p h c", c=64),
                                 rs[:, 0:nh].re("p (h o) -> p h o", o=1).tb([128, nh, 64]), ALU.mult)
                            if w_idx is not None:
                                K.tt("pool", dst.re("p (h c) -> p h c", c=64), dst.re("p (h c) -> p h c", c=64),
                                     whb[:, w_idx:w_idx + 1, :].tb([128, nh, 64]), ALU.mult)

                        def rope_apply(src, nh, t, dst, S=None):
                            S = S or SA[0]
                            t3 = S["t3"]
                            sv = src.re("p (h a s j) -> p h a s j", a=2, s=2, j=16)
                            dv = dst.re("p (h a s j) -> p h a s j", a=2, s=2, j=16)
                            cB = rope[:, t, 0:1, :].re("p o (a j) -> p o a j", a=2).tb([128, nh, 2, 16])
                            sB = rope[:, t, 1:2, :].re("p o (a j) -> p o a j", a=2).tb([128, nh, 2, 16])
                            a1 = t3[:, 0:nh * 32].re("p (h a j) -> p h a j", a=2, j=16)
                            a2 = t3[:, 256:256 + nh * 32].re("p (h a j) -> p h a j", a=2, j=16)
                            K.tt("pool", a1, sv[:, :, :, 0, :], cB, ALU.mult)
                            K.tt("dve", a2, sv[:, :, :, 1, :], sB, ALU.mult)
                            K.tt("dve", dv[:, :, :, 0, :], a1, a2, ALU.subtract)
                            K.tt("pool", a1, sv[:, :, :, 1, :], cB, ALU.mult)
                            K.tt("dve", a2, sv[:, :, :, 0, :], sB, ALU.mult)
                            K.tt("dve", dv[:, :, :, 1, :], a1, a2, ALU.add)

                        blocks = [[2 * b, 2 * b + 1] for b in range(9)]
                        wci = 0
                        ppi = 0
                        def hchain(blk):
                            if blk[0] in (0, 2):
                                bload(A1, 1, 1 if blk[0] == 0 else 0)
                                bload(SH1, 0, 1 if blk[0] == 0 else 0)
                            for bi, t in enumerate(blk):
                                ctx = 1 if t < 2 else 0
                                xt = xs.k(t)[:, t, :]
                                K.act(hbuf, xt, AF.Square, accum=ss[:, 0:1])
                                K.rstd(rs[:, 0:1], ss[:, 0:1], 1.0 / D, nhalf[:, 0:1])
                                K.stt("dve", hbuf, xt, rs[:, 0:1], A1, ALU.mult, ALU.mult)
                                K.tt("pool", hbuf, hbuf, SH1, ALU.add)
                                for half in range(2):
                                    for j in range(4):
                                        kc = half * 4 + j
                                        K.tr(ptr[:, j, :], hbuf[:, kc * 128:(kc + 1) * 128], ident)
                                    K.cp("act" if half == 0 else "dve",
                                         hTb[:, half * 4:half * 4 + 4, bi * 128:(bi + 1) * 128], ptr)
                        hchain(blocks[0])
                        for bix, blk in enumerate(blocks):
                            for c in range(5):
                                wpc = []
                                for hf in range(2):
                                    w = wc[wci % 2]
                                    wci += 1
                                    wpc.append(w)
                                    K.dma("sp", w, w_in[l, :, c * 512 + hf * 256:c * 512 + (hf + 1) * 256].re("(k p) n -> p k n", p=128))
                                pps = []
                                for bi, t in enumerate(blk):
                                    pps.append(ppA[ppi % 2])
                                    ppi += 1
                                for hf in range(2):
                                    for bi, t in enumerate(blk):
                                        for kc in range(8):
                                            K.mm(pps[bi][:, hf * 256:(hf + 1) * 256], hTb[:, kc, bi * 128:(bi + 1) * 128], wpc[hf][:, kc, :],
                                                 start=(kc == 0), stop=(kc == 7))
                                def post03(c, t, bi, pp, S):
                                    tok = slice(t * 128, (t + 1) * 128)
                                    zb = S["zb"]; t1 = S["t1"]; vnr = S["vnr"]; yab = S["yab"]; qrb = S["qrb"]
                                    qst = S["qst"]; ptrb = S["ptrb"]
                                    pmh = pmx[:, bi * 256:(bi + 1) * 256]
                                    if c == 0:
                                        K.act(zb, pp, AF.Gelu)
                                        headnorm(zb[:, 256:512], 4, None, t1[:, 0:256], S)
                                        K.cp("dve", vnr, t1[:, 0:256])
                                        for h in range(4):
                                            K.mm(pmh[:, h * 64:(h + 1) * 64], wsT[:, h, :], vnr[:, h * 64:(h + 1) * 64])
                                        for h in range(4):
                                            K.stt("dve", yab[:, h * 64:(h + 1) * 64], pmh[:, h * 64:(h + 1) * 64],
                                                  bsT[:, h:h + 1], zb[:, h * 64:(h + 1) * 64], ALU.add, ALU.mult)
                                        for j in range(2):
                                            K.tr(ptrb[:, j, :], yab[:, j * 128:(j + 1) * 128], identb)
                                        K.cp("act", catA.k(("a", t))[:, 0:2, tok], ptrb[:, 0:2, :])
                                    elif c == 1:
                                        K.cp("act", zb.re("p (j hi c) -> p hi j c", hi=2, j=4),
                                             pp.re("p (hi j c) -> p hi j c", hi=2, j=4))
                                        headnorm(zb, 8, 0, t1, S)
                                        rope_apply(t1, 8, t, qrb, S)
                                        for j in range(4):
                                            K.tr(ptrb[:, j, :], qrb[:, j * 128:(j + 1) * 128], identb)
                                        K.cp("act", qst, ptrb)
                                        K.dma("pool", qTD.k(t)[:, :, tok], qst)
                                    elif c == 2:
                                        K.cp("act", zb[:, 0:128], pp[:, 0:128])
                                        K.cp("act", Ve.k(t)[:, t, :, 0:64], pp[:, 128:256].re("p (h c) -> p h c", c=64))
                                        K.act(sqh[t % 2], pp[:, 256:512], AF.Silu)
                                        headnorm(zb[:, 0:128], 2, 1, t1[:, 0:128], S)
                                        rope_apply(t1[:, 0:128], 2, t, qrb[:, 0:128], S)
                                        K.tr(ptrb[:, 0, :], qrb[:, 0:128], identb)
                                        K.cp("act", kT.k(t)[:, tok], ptrb[:, 0, :])
                                    else:
                                        K.act(t1, pp, AF.Sigmoid)
                                        K.tt("dve", t1, t1, omlB, ALU.mult)
                                        K.tt("pool", t1, t1, lbB, ALU.add)
                                        K.act(logf[t % 2], t1, AF.Ln)
                                if c < 4:
                                    lists = []
                                    for bi, t in enumerate(blk):
                                        K.rec_start()
                                        post03(c, t, bi, pps[bi], SA[bi])
                                        lists.append(K.rec_stop())
                                    K.emit_interleaved(lists, lag=1)
                                    continue
                                def post4(t, bi, pp, S):
                                    hgt = S["hg"]; kkb = S["kkb"]; t2 = S["t2"]; t3 = S["t3"]; qeb = S["qeb"]; keb = S["keb"]
                                    pd = S["pd"]; ptrb = S["ptrb"]
                                    pmr = pmx[:, bi * 256:bi * 256 + 4]
                                    K.cp("act", hgt.k("v")[:, 2560:2816], pp[:, 0:256])
                                    K.act(kkb, logf[t % 2], AF.Exp)
                                    K.ts("dve", kkb, kkb, -1.0, 1.0, ALU.mult, ALU.add)
                                    K.act(hgt.k("v")[:, 2816:3072], pp[:, 256:512], AF.Silu)
                                    for d in range(2):
                                        base = d * 1280
                                        lf = logf[t % 2][:, d * 256:(d + 1) * 256]
                                        kd = kkb[:, d * 256:(d + 1) * 256]
                                        K.mm(pd[:, 0:256], hmat[:, 2 * d, :], lf)
                                        K.mm(pd[:, 256:512], hmat[:, 2 * d + 1, :], lf)
                                        for g in range(2):
                                            K.mm(pmr[:, 2 * g:2 * g + 2], lf[:, g * 128:(g + 1) * 128], ind)
                                        K.act(t2[:, 0:256], pd[:, 0:256], AF.Exp)
                                        K.tt("dve", qeb, sqh[t % 2], t2[:, 0:256], ALU.mult)
                                        K.act(t2[:, 256:512], pd[:, 0:256], AF.Exp, scale=-1.0)
                                        K.tt("pool", keb, kd, t2[:, 256:512], ALU.mult)
                                        K.act(t3[:, 0:256], pd[:, 256:512], AF.Exp)
                                        K.tt("dve", hgt.k("k2%d" % d)[:, base + 1024:base + 1280], kd, t3[:, 0:256], ALU.mult)
                                        tv = pmr.re("p (g c) -> p g c", c=2)
                                        K.act(EF.k((d, t))[:, d, :, 2 * t:2 * t + 2], tv, AF.Exp)
                                        K.act(EH.k((d, t))[:, d, :, 2 * t:2 * t + 2], tv, AF.Exp, scale=0.5)
                                        for g in range(2):
                                            K.tr(ptrb[:, g, :], qeb[:, g * 128:(g + 1) * 128], identb)
                                            K.tr(ptrb[:, 2 + g, :], keb[:, g * 128:(g + 1) * 128], identb)
                                        hq = hgt.k("q%d" % d)
                                        K.cp("act", hq[:, base:base + 256].re("p (g c) -> p g c", c=128), ptrb[:, 0:2, :])
                                        K.cp("dve", hq[:, base + 256:base + 512].re("p (g c) -> p g c", c=128)[:, :, 0:64],
                                             ptrb[:, 0:2, 0:64])
                                        K.cp("dve", hq[:, base + 512:base + 768].re("p (g c) -> p g c", c=128)[:, :, 64:128],
                                             ptrb[:, 0:2, 64:128])
                                        K.cp("act", hq[:, base + 768:base + 1024].re("p (g c) -> p g c", c=128), ptrb[:, 2:4, :])
                                    K.dma("pool", hgD.k(t)[t], hgt)
                                lists = []
                                for bi, t in enumerate(blk):
                                    K.rec_start()
                                    post4(t, bi, pps[bi], SA[bi])
                                    lists.append(K.rec_stop())
                                if bix + 1 < len(blocks):
                                    K.rec_start()
                                    hchain(blocks[bix + 1])
                                    lists.append(K.rec_stop())
                                K.emit_interleaved(lists, lag=0)
                        K.barrier()
                    catBC = K.sb(mix, "catBC", [128, 6, NTOK], BF16)
                    wob = K.sb(mix, "wob", [128, 8, D], BF16)
                    wstg = [K.sb(mix, "wstg%d" % i, [128, 4, D], F32) for i in range(2)]
                    if dbg is not None and dbg[0] == ("A", l):
                        with ExitStack() as ph:
                            d1 = K.sb(ph, "d1", [128, NTOK], F32)
                            d2 = K.sb(ph, "d2", [128, NTOK], BF16)
                            for i in range(4):
                                K.dma("sp", d2, qTD[:, i, :])
                                K.cp("dve", d1, d2)
                                dump(d1, i * 128)
                            K.cp("dve", d1, kT)
                            dump(d1, 512)
                            for i in range(2):
                                K.cp("dve", d1, catA[:, i, :])
                                dump(d1, 640 + i * 128)
                            K.barrier()
                        return nc
                    with ExitStack() as ph:
                        pT = [[K.sb(ph, "pT%d_%d" % (a, i), [128, 512], BF16) for i in range(3)] for a in range(2)]
                        osb = K.sb(ph, "osb", [128, 512], F32)
                        rr = K.sb(ph, "rr", [128, 512], F32)
                        sT = [[K.ps(ph, "sT%d_%d" % (a, i), [128, 512]) for i in range(2)] for a in range(2)]
                        oacc = [K.ps(ph, "oacc%d" % i, [128, 512]) for i in range(2)]
                        Rb = K.ps(ph, "Rb", [128, 512])
                        Vo = K.sb(ph, "Vo", [128, NT, 2, 128], BF16)
                        K.memset("pool", Vo, 0.0)
                        K.memset("pool", Vo[:, :, :, 0:1], 1.0)
                        for kvh_ in range(2):
                            K.cp("pool", Vo[:, :, kvh_, 64:128], Ve[:, :, kvh_, 0:64])
                        for half in range(2):
                            K.dma("sp", wstg[half], w_out[l, half * 512:(half + 1) * 512, :].re("(k p) n -> p k n", p=128))
                            K.cp("pool", wob[:, half * 4:half * 4 + 4, :], wstg[half])
                        qbl = [K.sb(ph, "qbl%d" % i, [128, 4, 512], BF16) for i in range(2)]
                        qblocks = [(0, 256, [0, 1])] + [(256 + 512 * b, 512, list(range(2, NT)) + [0, 1]) for b in range(4)]
                        if l == DEPTH - 1:
                            qblocks = qblocks[1:]
                        si_ = 0
                        for qbi, (tok0, N, stiles) in enumerate(qblocks):
                            qT = qbl[qbi % 2]
                            K.dma("sp", qT[:, :, 0:N], qTD[:, :, tok0:tok0 + N])
                            for j in range(4):
                                even = j % 2 == 0
                                nst = len(stiles)

                                def emit_acc(a, si, s, p_):
                                    if even:
                                        K.mm(oacc[a][0:65, 0:N], Ve[:, s, a, :], p_[:, 0:N], start=(si == 0), stop=(si == nst - 1))
                                    else:
                                        K.mm(oacc[a][:, 0:N], Vo[:, s, a, :], p_[:, 0:N], start=(si == 0), stop=(si == nst - 1))
                                pend = []
                                for si, s in enumerate(stiles):
                                    cur = []
                                    for a in range(2):
                                        st_ = sT[a][si_ % 2]
                                        K.mm(st_[:, 0:N], kT[a * 64:(a + 1) * 64, s * 128:(s + 1) * 128],
                                             qT[a * 64:(a + 1) * 64, j, 0:N])
                                    for a in range(2):
                                        st_ = sT[a][si_ % 2]
                                        p_ = pT[a][si_ % 3]
                                        K.act(p_[:, 0:N], st_[:, 0:N], AF.Exp, scale=0.125)
                                        cur.append((a, si, s, p_))
                                    si_ += 1
                                    pend.append(cur)
                                    if len(pend) > 1:
                                        for args in pend.pop(0):
                                            emit_acc(*args)
                                while pend:
                                    for args in pend.pop(0):
                                        emit_acc(*args)
                                for a in range(2):
                                    head = a * 4 + j
                                    acc = oacc[a]
                                    ch = 2 + head // 2
                                    if even:
                                        K.recip(rr[64:65, 0:N], acc[64:65, 0:N])
                                        K.mm(Rb[0:64, 0:N], onesf[64:65, 0:64], rr[64:65, 0:N])
                                        K.cp("act", osb[0:64, 0:N], acc[0:64, 0:N])
                                        K.tt("dve", catBC.k(("b", head, tok0))[0:64, ch - 2, tok0:tok0 + N], osb[0:64, 0:N],
                                             Rb[0:64, 0:N], ALU.mult)
                                    else:
                                        K.recip(rr[0:1, 0:N], acc[0:1, 0:N])
                                        K.mm(Rb[:, 0:N], onesf[0:1, :], rr[0:1, 0:N])
                                        K.cp("act", osb[64:128, 0:N], acc[64:128, 0:N])
                                        K.tt("dve", catBC.k(("b", head, tok0))[64:128, ch - 2, tok0:tok0 + N], osb[64:128, 0:N],
                                             Rb[64:128, 0:N], ALU.mult)
                        K.barrier()
                if dbg is not None and dbg[0] == ("B", l):
                    with ExitStack() as ph:
                        d1 = K.sb(ph, "d1", [128, NTOK], F32)
                        for i in range(4):
                            K.cp("dve", d1, catBC[:, i, :])
                            dump(d1, i * 128)
                        K.barrier()
                    return nc
                with ExitStack() as ph:
                    hgt2 = [K.sb(ph, "hgt%d" % i, [128, HGC], BF16) for i in range(2)]
                    oF = K.sb(ph, "oF", [128, NT, 256], F32)
                    S = [K.sb(ph, "S%d" % i, [128, 128], F32) for i in range(2)]
                    Sp = [[K.sb(ph, "Sp%d_%d" % (i, c), [128, 128], BF16) for c in range(2)] for i in range(2)]
                    ATm = [K.sb(ph, "ATm%d" % i, [128, 128], BF16) for i in range(4)]
                    osum = K.sb(ph, "osum", [128, 256], F32)
                    y1 = K.sb(ph, "y1", [128, 256], F32)
                    ycb = K.sb(ph, "ycb", [128, 256], BF16)
                    ss = K.sb(ph, "ssC", [128, 4], F32)
                    rs = K.sb(ph, "rsC", [128, 4], F32)
                    UpsB = [K.ps(ph, "Ups%d" % i, [128, 4, 128]) for i in range(2)]
                    ATpB = [K.ps(ph, "ATp%d" % i, [128, 4, 128]) for i in range(2)]
                    ops_ = [K.ps(ph, "ops%d" % i, [128, 512]) for i in range(2)]
                    ptrb = K.ps(ph, "ptrbC", [128, 8, 128], BF16)
                    li = 0
                    for d in range(2):
                        base = d * 1280
                        order = list(range(NT)) if d == 0 else [1, 0] + list(range(NT - 1, 1, -1))
                        corder = (0, 1) if d == 0 else (1, 0)
                        for hp in range(2):
                            K.memset("pool", S[hp], 0.0)
                        for t in order:
                            hgt = hgt2[li % 2]
                            o_ps = ops_[li % 2]
                            li += 1
                            K.dma("sp", hgt, hgD.k(t)[t])
                            tok = slice(t * 128, (t + 1) * 128)
                            SK = os.environ.get("KSKIP", "")
                            for hp in range(2):
                                for c in range(2):
                                    if "U" in SK:
                                        continue
                                    if "c0" in SK and c == 1:
                                        continue
                                    K.mm(UpsB[c][:, hp, :],
                                         hgt[c * 64:(c + 1) * 64, base + 1024 + hp * 128:base + 1024 + (hp + 1) * 128],
                                         hgt[c * 64:(c + 1) * 64, 2560 + hp * 128:2560 + (hp + 1) * 128])
                                for h in range(2):
                                    head = hp * 2 + h
                                    if "AT" in SK:
                                        continue
                                    K.mm(ATpB[h][:, hp, :],
                                         hgt[h * 64:(h + 1) * 64, base + 768 + hp * 128:base + 768 + (hp + 1) * 128],
                                         hgt[h * 64:(h + 1) * 64, base + hp * 128:base + (hp + 1) * 128])
                                    K.cp("act", ATm[head], ATpB[h][:, hp, :])
                                    if "asel" in os.environ.get("KSKIP", ""):
                                        pass
                                    elif d == 0:
                                        K.asel(ATm[head], [[1, 128]], -1, ALU.is_ge)
                                        K.memset("pool", ATm[head][0:64, 64:128], 0.0)
                                    else:
                                        K.asel(ATm[head], [[-1, 128]], 1, ALU.is_ge)
                                        K.memset("pool", ATm[head][64:128, 0:64], 0.0)
                                for c in corder:
                                    ci = 2 * t + c
                                    if "chain" in os.environ.get("KSKIP", ""):
                                        continue
                                    K.ts("dve", Sp[hp][c], S[hp], EH[:, d, hp, ci:ci + 1], None, ALU.mult)
                                    K.stt("dve", S[hp], S[hp], EF[:, d, hp, ci:ci + 1], UpsB[c][:, hp, :], ALU.mult, ALU.add)
                                for h in range(2):
                                    head = hp * 2 + h
                                    if "omm" in SK:
                                        continue
                                    oo = o_ps[:, head * 64:(head + 1) * 64]
                                    K.mm(oo, ATm[head], hgt[:, 2560 + head * 64:2560 + (head + 1) * 64], start=True, stop=False)
                                    if "inter" in os.environ.get("KSKIP", ""):
                                        K.mm(oo, ATm[head], hgt[:, 2560 + head * 64:2560 + (head + 1) * 64], start=False, stop=True)
                                        continue
                                    for ic, c in enumerate(corder):
                                        qz = base + 256 + c * 256 + hp * 128
                                        K.mm(oo, hgt[h * 64:(h + 1) * 64, qz:qz + 128],
                                             Sp[hp][c][h * 64:(h + 1) * 64, h * 64:(h + 1) * 64], start=False, stop=(ic == 1))
                            if "omm" in SK:
                                pass
                            elif d == 0:
                                K.cp("act", oF.k(t)[:, t, :], o_ps[:, 0:256])
                            elif "post" in os.environ.get("KSKIP", ""):
                                K.cp("act", oF.k(t)[:, t, :], o_ps[:, 0:256])
                            else:
                                K.tt("dve", osum, o_ps[:, 0:256], oF.k(t)[:, t, :], ALU.add)
                                K.tt("pool", y1, osum, osum, ALU.mult)
                                K.red("dve", ss, y1.re("p (h c) -> p h c", c=64))
                                K.act(rs, ss, AF.Sqrt, bias=epsc, scale=1.0 / 64)
                                K.recip(rs, rs)
                                K.tt("dve", y1.re("p (h c) -> p h c", c=64), osum.re("p (h c) -> p h c", c=64),
                                     rs.re("p (h o) -> p h o", o=1).tb([128, 4, 64]), ALU.mult)
                                K.tt("pool", y1.re("p (h c) -> p h c", c=64), y1.re("p (h c) -> p h c", c=64),
                                     whb[:, 2:3, :].tb([128, 4, 64]), ALU.mult)
                                K.tt("dve", ycb, y1, hgt[:, 2816:3072], ALU.mult)
                                for g in range(2):
                                    K.tr(ptrb[:, g, :], ycb[:, g * 128:(g + 1) * 128], identb)
                                K.cp("act", catBC.k(("c", t))[:, 4:6, tok], ptrb[:, 0:2, :])
                    K.barrier()
                if dbg is not None and dbg[0] == ("C", l):
                    with ExitStack() as ph:
                        d1 = K.sb(ph, "d1", [128, NTOK], F32)
                        for i in range(2):
                            K.cp("dve", d1, catBC[:, 4 + i, :])
                            dump(d1, i * 128)
                        K.barrier()
                    return nc
                with ExitStack() as ph:
                    G1 = K.sb(ph, "G1", [128, 2, D], F32)
                    tmp = [K.sb(ph, "tmpD%d" % i, [128, 512], F32) for i in range(2)]
                    pso = [K.ps(ph, "pso%d" % i, [128, 512]) for i in range(4)]
                    for ctx in range(2):
                        bload(G1[:, ctx, :], 2, ctx)
                    i = 0
                    for t in tiles:
                        ctx = 1 if t < 2 else 0
                        for half in range(2):
                            pp = pso[i % 4]
                            tm = tmp[i % 2]
                            i += 1
                            for kc in range(8):
                                K.mm(pp, (catA[:, kc, t * 128:(t + 1) * 128] if kc < 2 else catBC[:, kc - 2, t * 128:(t + 1) * 128]), wob[:, kc, half * 512:(half + 1) * 512],
                                     start=(kc == 0), stop=(kc == 7))
                            xv = xs.k(t)[:, t, half * 512:(half + 1) * 512]
                            K.tt("dve", tm, pp, G1[:, ctx, half * 512:(half + 1) * 512], ALU.mult)
                            K.tt("pool", xv, xv, tm, ALU.add)
                    K.barrier()
            if dbg is not None and dbg[0] == ("D", l):
                for t in range(NT):
                    dump(xs[:, t, :], t * 128)
                K.barrier()
                return nc
            etiles = [t for t in tiles if t >= 2] + [t for t in tiles if t < 2]
            with ExitStack() as eph:
                igat = K.sb(eph, "igat", [128, NT, 2], U32)
                gts = K.sb(eph, "gts", [128, NT, 2], F32)
                ring = [K.sb(eph, "ring%d" % i, [128, 4096], F32R) for i in range(4)]
                cnt_ = {"ri": 0, "xi": 0, "yi": 0, "pi": 0}
                wv = {}

                def stageW(e):
                    ri = cnt_["ri"]
                    cnt_["ri"] += 3
                    w1v = ring[ri % 4].re("p (k n) -> p k n", k=8)
                    w3v = ring[(ri + 1) % 4].re("p (k n) -> p k n", k=8)
                    w2v = ring[(ri + 2) % 4].re("p (k n) -> p k n", k=4)
                    K.dma("sp", w1v, w1[l, e].re("(k p) n -> p k n", p=128))
                    K.dma("sp", w3v, w3[l, e].re("(k p) n -> p k n", p=128))
                    K.dma("sp", w2v, w2[l, e].re("(k p) n -> p k n", p=128))
                    wv[e] = (w1v, w3v, w2v)
                with ExitStack() as ph:
                    A2 = K.sb(ph, "A2", [128, D], F32)
                    SH2 = K.sb(ph, "SH2", [128, D], F32)
                    wr = K.sb(ph, "wr", [128, 8, 36], F32)
                    brb = K.sb(ph, "brb", [128, 36], F32)
                    K.dma("sp", wr, w_r[l].re("(k p) n -> p k n", p=128))
                    K.dma("sp", brb, b_r[l:l + 1, :].tb([128, 36]))
                    stageW(0)
                    ecr = K.sb(ph, "ecr", [128, 33], F32)
                    K.dma("sp", ecr, c_ecr)
                    ltm = K.sb(ph, "ltm", [128, 128], F32)
                    K.dma("sp", ltm, c_ltm)
                    baseb = K.sb(ph, "baseb", [128, 32], F32)
                    K.memset("pool", baseb, 0.0)
                    SC = []
                    for q in range(2):
                        sc = {}
                        sc["h2"] = K.sb(ph, "h2_%d" % q, [128, D], F32)
                        sc["h2Tf"] = K.sb(ph, "h2Tf%d" % q, [128, 8, 128], F32)
                        sc["h2h"] = K.sb(ph, "h2h%d" % q, [128, D], BF16)
                        for nm, w in (("lg", 36), ("em", 32), ("sm", 24), ("t8", 8), ("oh", 4), ("s1", 32), ("s2", 32),
                                      ("aa", 32), ("pos", 32), ("vv", 32), ("tq", 32)):
                            sc[nm] = K.sb(ph, "%s%d" % (nm, q), [128, w], F32)
                        sc["isx"] = K.sb(ph, "isc%d" % q, [128, 2], U32)
                        sc["ptr"] = [K.ps(ph, "ptrE%d_%d" % (q, i), [128, 4, 128], F32) for i in range(2)]
                        sc["plg"] = K.ps(ph, "plg%d" % q, [128, 64])
                        sc["pcs"] = K.ps(ph, "pcs%d" % q, [128, 64])
                        SC.append(sc)
                    cur_ctx = [None]

                    def setmod(ctx):
                        if cur_ctx[0] != ctx:
                            cur_ctx[0] = ctx
                            bload(A2, 4, ctx)
                            bload(SH2, 3, ctx)

                    def e1_tile(ti, t, sc):
                        h2 = sc["h2"]; h2Tf = sc["h2Tf"]; lg = sc["lg"]; em = sc["em"]; sm = sc["sm"]; t8 = sc["t8"]
                        oh = sc["oh"]; s1 = sc["s1"]; s2 = sc["s2"]; aa = sc["aa"]; pos = sc["pos"]; vv = sc["vv"]
                        tq = sc["tq"]; isx = sc["isx"]; ptr = sc["ptr"]; plg = sc["plg"]; pcs = sc["pcs"]; h2h = sc["h2h"]
                        setmod(1 if t < 2 else 0)
                        xt = xs.k(t)[:, t, :]
                        K.act(h2, xt, AF.Square, accum=sm[:, 0:1])
                        K.act(sm[:, 1:2], sm[:, 0:1], AF.Sqrt, bias=epsc, scale=1.0 / D)
                        K.recip(sm[:, 1:2], sm[:, 1:2])
                        K.stt("dve", h2, xt, sm[:, 1:2], A2, ALU.mult, ALU.mult)
                        K.tt("pool", h2, h2, SH2, ALU.add)
                        K.cp("pool", h2h, h2)
                        for half in range(2):
                            for j in range(4):
                                kc = half * 4 + j
                                K.tr(ptr[half][:, j, :], h2[:, kc * 128:(kc + 1) * 128], ident)
                            K.cp("act", h2Tf[:, half * 4:half * 4 + 4, :], ptr[half])
                        for kc in range(8):
                            K.mm(plg[:, 0:36], h2Tf[:, kc, :], wr[:, kc, :], start=(kc == 0), stop=(kc == 7))
                        K.tt("dve", lg, plg[:, 0:36], brb, ALU.add)
                        K.red("dve", sm[:, 2:3], lg[:, 0:4], ALU.max)
                        K.ts("dve", oh, lg[:, 0:4], sm[:, 2:3], None, ALU.is_equal)
                        K.ts("dve", sm[:, 3:4], sm[:, 2:3], -1.0, None, ALU.mult)
                        K.act(s1[:, 0:4], lg[:, 0:4], AF.Exp, bias=sm[:, 3:4], accum=sm[:, 4:5])
                        K.recip(sm[:, 5:6], sm[:, 4:5])
                        K.ts("dve", oh, oh, 1e30, -1e30, ALU.mult, ALU.add)
                        K.tt("dve", em.re("p (g c) -> p g c", c=8), lg[:, 4:36].re("p (g c) -> p g c", c=8),
                             oh.re("p (g o) -> p g o", o=1).tb([128, 4, 8]), ALU.add)
                        K.top8(t8, em)
                        K.ts("dve", s1, em, t8[:, 0:1], None, ALU.is_equal)
                        K.ts("dve", s2, em, t8[:, 1:2], None, ALU.is_equal)
                        K.tt("dve", sm[:, 6:7], t8[:, 1:2], t8[:, 0:1], ALU.subtract)
                        K.act(sm[:, 7:8], sm[:, 6:7], AF.Exp)
                        K.ts("dve", sm[:, 7:8], sm[:, 7:8], 1.0, None, ALU.add)
                        K.recip(sm[:, 8:9], sm[:, 7:8])
                        K.ts("dve", sm[:, 9:10], sm[:, 8:9], -1.0, 1.0, ALU.mult, ALU.add)
                        K.tt("dve", sm[:, 8:10], sm[:, 8:10], sm[:, 5:6].tb([128, 2]), ALU.mult)
                        K.tt("dve", aa, s1, s2, ALU.add)
                        K.mm(pcs[:, 0:32], ltm, aa)
                        K.mm(pcs[:, 32:64], onesf, aa)
                        K.tt("dve", pos, pcs[:, 0:32], baseb, ALU.add)
                        K.tt("dve", baseb, pcs[:, 32:64], baseb, ALU.add)
                        K.tt("pool", vv, pos, ecr[:, 0:32], ALU.add)
                        for k, sk in enumerate((s1, s2)):
                            c0 = 10 + 6 * k
                            K.tt("dve", tq, pos, sk, ALU.mult)
                            K.red("dve", sm[:, c0:c0 + 1], tq)
                            K.tt("dve", tq, vv, sk, ALU.mult)
                            K.red("dve", sm[:, c0 + 1:c0 + 2], tq)
                            K.ts("dve", sm[:, c0 + 2:c0 + 3], sm[:, c0:c0 + 1], float(CAP), None, ALU.is_lt)
                            K.ts("dve", sm[:, c0 + 3:c0 + 4], ecr[:, 32:33], float(NE * CAP + (ti * 2 + k) * 128), None, ALU.add)
                            K.tt("dve", sm[:, c0 + 4:c0 + 5], sm[:, c0 + 1:c0 + 2], sm[:, c0 + 3:c0 + 4], ALU.subtract)
                            K.stt("dve", sm[:, c0 + 4:c0 + 5], sm[:, c0 + 4:c0 + 5], sm[:, c0 + 2:c0 + 3], sm[:, c0 + 3:c0 + 4],
                                  ALU.mult, ALU.add)
                            K.cp("dve", isx[:, k:k + 1], sm[:, c0 + 4:c0 + 5])
                            K.tt("dve", sm[:, c0 + 5:c0 + 6], sm[:, c0 + 1:c0 + 2], sm[:, c0 + 2:c0 + 3], ALU.mult)
                            K.cp("dve", igat.k(t)[:, t, k:k + 1], sm[:, c0 + 5:c0 + 6])
                            K.tt("dve", gts.k(t)[:, t, k:k + 1], sm[:, 8 + k:9 + k], sm[:, c0 + 2:c0 + 3], ALU.mult)
                            K.op("pool", (lambda e, isx=isx, k=k, h2h=h2h: e.indirect_dma_start(
                                out=XS.ap, out_offset=bass.IndirectOffsetOnAxis(isx.ap[:, k:k + 1], 0),
                                in_=h2h.ap, in_offset=None)), [h2h, isx], [XS.k(("s", ti, k))], dma=True)
                    for p_ in range(0, len(etiles), 2):
                        lists = []
                        for q in range(2):
                            if p_ + q < len(etiles):
                                K.rec_start()
                                e1_tile(p_ + q, etiles[p_ + q], SC[q])
                                lists.append(K.rec_stop())
                        K.emit_interleaved(lists)
                    K.barrier()
                with ExitStack() as ph:
                    XTb = [K.sb(ph, "XT%d" % i, [128, 8, CAP], F32R) for i in range(2)]
                    hTgb = [K.sb(ph, "hTg%d" % i, [128, 4, CAP], F32R) for i in range(2)]
                    xe = [K.sb(ph, "xe%d" % i, [128, D], BF16) for i in range(3)]
                    yst = [K.sb(ph, "yst%d" % i, [128, D], BF16) for i in range(3)]
                    sa = [K.sb(ph, "sa%d" % i, [128, CAP], F32) for i in range(2)]
                    ptr = [K.ps(ph, "ptrX%d" % i, [128, 4, 128], BF16) for i in range(2)]
                    pab = [K.ps(ph, "pab%d" % i, [128, 512]) for i in range(4)]
                    py = [K.ps(ph, "py%d" % i, [128, 512]) for i in range(2)]
                    if os.environ.get("KVERB"):
                        print("phase E2 sbuf remaining", nc.sbuf_bytes_remaining, flush=True)
                    NCT = CAP // 128
                    def stageX(e):
                        XT = XTb[e % 2]
                        for ct in range(NCT):
                            x_ = xe[cnt_["xi"] % 3]
                            cnt_["xi"] += 1
                            K.dma("sp", x_, XS[e * CAP + ct * 128:e * CAP + (ct + 1) * 128, :])
                            for half in range(2):
                                for j in range(4):
                                    kc = half * 4 + j
                                    K.tr(ptr[half][:, j, :], x_[:, kc * 128:(kc + 1) * 128], identb)
                                K.cp("act" if half == 0 else "dve", XT[:, half * 4:half * 4 + 4, ct * 128:(ct + 1) * 128], ptr[half])

                    def stageAB(e):
                        XT = XTb[e % 2]
                        hTg = hTgb[e % 2]
                        w1v, w3v, w2v = wv[e]
                        for ffc in range(4):
                            i = cnt_["pi"]
                            cnt_["pi"] += 2
                            pa = pab[i % 4]
                            pb = pab[(i + 1) % 4]
                            sa_ = sa[(i // 2) % 2]
                            for kc in range(8):
                                K.mm(pa[:, 0:CAP], w1v[:, kc, ffc * 128:(ffc + 1) * 128], XT[:, kc, :],
                                     start=(kc == 0), stop=(kc == 7))
                            for kc in range(8):
                                K.mm(pb[:, 0:CAP], w3v[:, kc, ffc * 128:(ffc + 1) * 128], XT[:, kc, :],
                                     start=(kc == 0), stop=(kc == 7))
                            K.act(sa_, pa[:, 0:CAP], AF.Silu)
                            K.tt("dve", hTg[:, ffc, :], sa_, pb[:, 0:CAP], ALU.mult)

                    def stageY(e):
                        hTg = hTgb[e % 2]
                        w1v, w3v, w2v = wv[e]
                        for ct in range(NCT):
                            y_ = yst[cnt_["yi"] % 3]
                            cnt_["yi"] += 1
                            for half in range(2):
                                pyy = py[half]
                                for ffc in range(4):
                                    K.mm(pyy, hTg[:, ffc, ct * 128:(ct + 1) * 128], w2v[:, ffc, half * 512:(half + 1) * 512],
                                         start=(ffc == 0), stop=(ffc == 3))
                                K.cp("act" if half == 0 else "dve", y_[:, half * 512:(half + 1) * 512], pyy)
                            K.dma("pool", YS.k((e, ct))[e * CAP + ct * 128:e * CAP + (ct + 1) * 128, :], y_)

                    if "noE2" not in os.environ.get("KSKIP", ""):
                        stageX(0)
                        for e in range(NE):
                            stageAB(e)
                            if e + 1 < NE:
                                stageX(e + 1)
                                stageW(e + 1)
                            stageY(e)
                    K.barrier()
                with ExitStack() as ph:
                    G2m = [K.sb(ph, "G2_%d" % i, [128, D], F32) for i in range(2)]
                    for ctx in range(2):
                        bload(G2m[ctx], 5, ctx)
                    yg = [[K.sb(ph, "yg%d_%d" % (i, k), [128, D], BF16) for k in range(2)] for i in range(3)]
                    mt = K.sb(ph, "mt", [128, D], F32)
                    for ti, t in enumerate(etiles):
                        if "noE3" in os.environ.get("KSKIP", ""):
                            continue
                        ctx = 1 if t < 2 else 0
                        ya_, yb_ = yg[ti % 3]
                        for k, yy in enumerate((ya_, yb_)):
                            K.op("pool", (lambda e, yy=yy, t=t, k=k: e.indirect_dma_start(
                                out=yy.ap, out_offset=None, in_=YS.ap,
                                in_offset=bass.IndirectOffsetOnAxis(igat.ap[:, t, k:k + 1], 0))), [YS, igat], [yy], dma=True)
                        K.ts("dve", mt, ya_, gts[:, t, 0:1], None, ALU.mult)
                        K.stt("dve", mt, yb_, gts[:, t, 1:2], mt, ALU.mult, ALU.add)
                        K.tt("dve", mt, mt, G2m[ctx], ALU.mult)
                        xv = xs.k(t)[:, t, :]
                        K.tt("dve", xv, xv, mt, ALU.add)
                    K.barrier()
            if dbg is not None and dbg[0] == ("E", l):
                for t in range(NT):
                    dump(xs[:, t, :], t * 128)
                K.barrier()
                return nc
        for t in range(2, NT):
            K.dma("sp", out[(t - 2) * 128:(t - 1) * 128, :], xs.k(t)[:, t, :])
        K.barrier()
    return nc


def make_in_maps(inputs, n_cores=8):
    f = lambda a: np.ascontiguousarray(np.asarray(a, dtype=np.float32))
    x = f(inputs["x"]); c = f(inputs["c"]); ctx = f(inputs["ctx"]); c_ctx = f(inputs["c_ctx"])
    consts = host_consts()
    shared = {
        "w_mod": f(inputs["w_mod"]), "b_mod": f(inputs["b_mod"]),
        "norm1_w": f(inputs["norm1_w"]), "norm2_w": f(inputs["norm2_w"]),
        "w_in": f(inputs["w_in"]), "w_s": f(inputs["w_s"]),
        "b_sT": f(np.transpose(inputs["b_s"], (0, 2, 1))),
        "qkh_w": f(np.stack([inputs["q_norm_w"], inputs["k_norm_w"], inputs["hgrn_norm_w"]], axis=1)),
        "lbl": f(np.asarray(inputs["hgrn_lb_logits"]).reshape(DEPTH, 512)),
        "w_out": f(inputs["w_out"]),
        "w_r": f(np.concatenate([inputs["w_grp"], inputs["w_exp"]], axis=2)),
        "b_r": f(np.concatenate([inputs["b_grp"], inputs["b_exp"]], axis=1)),
        "w1": f(inputs["w1"]), "w3": f(inputs["w3"]), "w2": f(inputs["w2"]),
    }
    shared.update(consts)
    maps = []
    for b in range(n_cores):
        m = dict(shared)
        m["xin"] = np.ascontiguousarray(np.concatenate([ctx[b], x[b]], axis=0))
        cc = np.stack([c[b], c_ctx], axis=1)
        m["ccT"] = np.ascontiguousarray(cc.reshape(8, 128, 2).transpose(1, 0, 2))
        maps.append(m)
    return maps


def kernel(**inputs):
    nc = bass.Bass("TRN2", target_bir_lowering=False)
    build(nc)
    maps = make_in_maps(inputs, 8)
    res = run_bass_kernel_spmd(nc, maps, core_ids=list(range(8)))
    return np.stack([np.asarray(r["out"], dtype=np.float32) for r in res.results], axis=0)
```
